# Optimizing a Trainium2 kernel written in Bass

```python
import jax, jax.numpy as jnp
from jax import lax
import numpy as np

D_MODEL = 2048
BATCH = 4
SEQ = 4096
DEPTH = 1
DEC_BATCH = 1
DEC_SEQ = 8192
PAST_LEN = 128

GRID_W = 64
D_POOL = 1024
POOL_WINDOWS = (2, 4, 8, 16)
N_POOL_GROUPS = len(POOL_WINDOWS)
POOL_GROUP = D_POOL // N_POOL_GROUPS
D_NA = D_MODEL - D_POOL
NA_HEADS = 16
NA_HEAD_DIM = D_NA // NA_HEADS
NA_ROWS_MAX = 8
NA_COLS = 16
D_IN = D_POOL + 3 * D_NA
N_MEM = 256
XA_HEADS = 4
XA_HEAD_DIM = D_MODEL // XA_HEADS
N_EXPERTS = 32
TOP_K = 4
D_EXPERT = D_MODEL
SWIGLU_LIMIT = 7.0
SWIGLU_ALPHA = 1.702
MOE_BLOCK = 256
LN_EPS = 1e-5
DEEPNORM_ALPHA = (2 * DEPTH) ** 0.25
DEEPNORM_BETA = (8 * DEPTH) ** -0.25
NEG_INF = -1e30

kernel_name = "hybrid_pool_natten_moe_encoder"


def layer_norm(x, g, b):
    xf = x.astype(jnp.float32)
    mu = jnp.mean(xf, axis=-1, keepdims=True)
    xc = xf - mu
    var = jnp.mean(xc * xc, axis=-1, keepdims=True)
    y = xc * lax.rsqrt(var + LN_EPS) * g.astype(jnp.float32) + b.astype(jnp.float32)
    return y.astype(x.dtype)


def multiscale_pool(u, w_pool, pool_scale):
    B, L, _ = u.shape
    uf = u.astype(jnp.float32)
    cs = jnp.concatenate([jnp.zeros((B, 1, D_POOL), jnp.float32), jnp.cumsum(uf, axis=1)], axis=1)
    t = jnp.arange(L)
    outs = []
    for g, w in enumerate(POOL_WINDOWS):
        lo = jnp.clip(t - w // 2, 0, L)
        hi = jnp.clip(t - w // 2 + w, 0, L)
        sl = slice(g * POOL_GROUP, (g + 1) * POOL_GROUP)
        csg = cs[:, :, sl]
        mean = (csg[:, hi] - csg[:, lo]) / (hi - lo).astype(jnp.float32)[None, :, None]
        outs.append(mean - uf[:, :, sl])
    p = jnp.stack(outs, axis=2).astype(u.dtype)
    y = jnp.einsum('blgc,gcd->blgd', p, w_pool).reshape(B, L, D_POOL)
    return y * pool_scale


def neighbourhood_attention(q, k, v, rpb):
    B, L = q.shape[0], q.shape[1]
    rows = L // GRID_W
    kr = min(NA_ROWS_MAX, rows)
    to_grid = lambda a: a.reshape(B, rows, GRID_W, NA_HEADS, NA_HEAD_DIM)
    qg, kg, vg = to_grid(q), to_grid(k), to_grid(v)
    r = jnp.arange(rows)
    row_start = jnp.clip(r - kr // 2, 0, rows - kr)
    key_rows = row_start[:, None] + jnp.arange(kr)[None, :]
    kb = kg[:, key_rows]
    vb = vg[:, key_rows]
    c = jnp.arange(GRID_W)
    col_start = jnp.clip(c - NA_COLS // 2, 0, GRID_W - NA_COLS)
    col_off = c[None, :] - col_start[:, None]
    col_mask = (col_off >= 0) & (col_off < NA_COLS)
    dr_idx = key_rows - r[:, None] + NA_ROWS_MAX - 1
    dc_idx = jnp.clip(c[None, :] - c[:, None] + NA_COLS - 1, 0, 2 * NA_COLS - 2)
    bias = rpb[:, dr_idx[:, :, None, None], dc_idx[None, None]].astype(jnp.float32)
    bias = jnp.where(col_mask[None, None, None], bias, NEG_INF)
    bias = bias.transpose(0, 1, 3, 2, 4)
    s = jnp.einsum('brqhd,brjkhd->bhrqjk', qg, kb).astype(jnp.float32) * (NA_HEAD_DIM ** -0.5)
    s = s + bias[None]
    p = jax.nn.softmax(s.reshape(B, NA_HEADS, rows, GRID_W, kr * GRID_W), axis=-1)
    p = p.reshape(s.shape).astype(v.dtype)
    o = jnp.einsum('bhrqjk,brjkhd->brqhd', p, vb)
    return o.reshape(B, L, D_NA)


def memory_cross_attention(h, mem, w_xq, w_xkv, w_xo):
    B, L, _ = h.shape
    q = (h @ w_xq).reshape(B, L, XA_HEADS, XA_HEAD_DIM)
    kv = (mem @ w_xkv).reshape(B, mem.shape[1], 2, XA_HEADS, XA_HEAD_DIM)
    k, v = kv[:, :, 0], kv[:, :, 1]
    s = jnp.einsum('blhd,bmhd->bhlm', q, k).astype(jnp.float32) * (XA_HEAD_DIM ** -0.5)
    p = jax.nn.softmax(s, axis=-1).astype(h.dtype)
    o = jnp.einsum('bhlm,bmhd->blhd', p, v).reshape(B, L, D_MODEL)
    return o @ w_xo


def routed_experts(h, w_router, b_router, w_gu, b_gu, w_down, b_down):
    T = h.shape[0]
    TK = T * TOP_K
    logits = (h @ w_router).astype(jnp.float32) + b_router.astype(jnp.float32)
    top_v, top_i = lax.top_k(logits, TOP_K)
    gates = jax.nn.softmax(top_v, axis=-1)
    flat_e = top_i.reshape(TK).astype(jnp.int32)
    flat_g = gates.reshape(TK)
    flat_tok = jnp.arange(TK, dtype=jnp.int32) // TOP_K
    counts = jnp.bincount(flat_e, length=N_EXPERTS)
    padded = (counts + MOE_BLOCK - 1) // MOE_BLOCK * MOE_BLOCK
    padded_end = jnp.cumsum(padded)
    padded_start = padded_end - padded
    group_start = jnp.cumsum(counts) - counts
    order = jnp.argsort(flat_e)
    sorted_e = flat_e[order]
    dest = padded_start[sorted_e] + (jnp.arange(TK) - group_start[sorted_e])
    n_slots = TK + N_EXPERTS * MOE_BLOCK
    n_blocks = n_slots // MOE_BLOCK
    slot_tok = jnp.full((n_slots,), T, jnp.int32).at[dest].set(flat_tok[order])
    slot_gate = jnp.zeros((n_slots,), jnp.float32).at[dest].set(flat_g[order])
    block_expert = jnp.clip(jnp.searchsorted(padded_end, jnp.arange(n_blocks) * MOE_BLOCK, side='right'),
                            0, N_EXPERTS - 1)
    h_pad = jnp.concatenate([h, jnp.zeros((1, D_MODEL), h.dtype)], axis=0)
    xs = h_pad[slot_tok].reshape(n_blocks, MOE_BLOCK, D_MODEL)

    def expert_block(args):
        xb, e = args
        gu = xb @ w_gu[e] + b_gu[e]
        gate = jnp.minimum(gu[:, :D_EXPERT], SWIGLU_LIMIT)
        up = jnp.clip(gu[:, D_EXPERT:], -SWIGLU_LIMIT, SWIGLU_LIMIT)
        act = gate * jax.nn.sigmoid(SWIGLU_ALPHA * gate) * (up + 1)
        return act @ w_down[e] + b_down[e]

    out = lax.map(expert_block, (xs, block_expert)).reshape(n_slots, D_MODEL)
    out = out.astype(jnp.float32) * slot_gate[:, None]
    y = jax.ops.segment_sum(out, slot_tok, num_segments=T + 1)[:T]
    return y.astype(h.dtype)


def encoder_layer(x, mem, w_in, w_pool, pool_scale, rpb, w_out, ln1_g, ln1_b,
                  w_xq, w_xkv, w_xo, ln2_g, ln2_b,
                  w_router, b_router, w_gu, b_gu, w_down, b_down, ln3_g, ln3_b):
    B, L, _ = x.shape
    u = x @ w_in
    a = u[..., :D_POOL]
    qkv = u[..., D_POOL:].reshape(B, L, 3, NA_HEADS, NA_HEAD_DIM)
    ya = multiscale_pool(a, w_pool, pool_scale)
    yb = neighbourhood_attention(qkv[:, :, 0], qkv[:, :, 1], qkv[:, :, 2], rpb)
    mix = jnp.concatenate([ya, yb], axis=-1) @ w_out
    x = layer_norm(DEEPNORM_ALPHA * x + mix, ln1_g, ln1_b)
    x = layer_norm(DEEPNORM_ALPHA * x + memory_cross_attention(x, mem, w_xq, w_xkv, w_xo), ln2_g, ln2_b)
    h = routed_experts(x.reshape(B * L, D_MODEL), w_router, b_router, w_gu, b_gu, w_down, b_down)
    x = layer_norm(DEEPNORM_ALPHA * x + h.reshape(B, L, D_MODEL), ln3_g, ln3_b)
    return x


def setup_inputs(seed: int = 0) -> dict:
    key = jax.random.key(seed)
    ks = jax.random.split(key, 32)
    nrm = lambda k, shape, s: jax.random.normal(k, shape, jnp.float32) * s
    return {
        "x_prompt": nrm(ks[0], (BATCH, SEQ, D_MODEL), 1.0),
        "x_sample": nrm(ks[1], (DEC_BATCH, DEC_SEQ, D_MODEL), 1.0),
        "mem_prompt": nrm(ks[2], (BATCH, N_MEM, D_MODEL), 1.0),
        "mem_sample": nrm(ks[3], (DEC_BATCH, N_MEM, D_MODEL), 1.0),
        "w_in": nrm(ks[4], (DEPTH, D_MODEL, D_IN), D_MODEL ** -0.5),
        "w_pool": nrm(ks[5], (DEPTH, N_POOL_GROUPS, POOL_GROUP, POOL_GROUP), POOL_GROUP ** -0.5),
        "pool_scale": 1.0 + nrm(ks[6], (DEPTH, D_POOL), 0.1),
        "rpb": nrm(ks[7], (DEPTH, NA_HEADS, 2 * NA_ROWS_MAX - 1, 2 * NA_COLS - 1), 0.1),
        "w_out": nrm(ks[8], (DEPTH, D_MODEL, D_MODEL), D_MODEL ** -0.5 * DEEPNORM_BETA),
        "ln1_g": 1.0 + nrm(ks[9], (DEPTH, D_MODEL), 0.05),
        "ln1_b": nrm(ks[10], (DEPTH, D_MODEL), 0.02),
        "w_xq": nrm(ks[11], (DEPTH, D_MODEL, D_MODEL), D_MODEL ** -0.5),
        "w_xkv": nrm(ks[12], (DEPTH, D_MODEL, 2 * D_MODEL), D_MODEL ** -0.5),
        "w_xo": nrm(ks[13], (DEPTH, D_MODEL, D_MODEL), D_MODEL ** -0.5 * DEEPNORM_BETA),
        "ln2_g": 1.0 + nrm(ks[14], (DEPTH, D_MODEL), 0.05),
        "ln2_b": nrm(ks[15], (DEPTH, D_MODEL), 0.02),
        "w_router": nrm(ks[16], (DEPTH, D_MODEL, N_EXPERTS), D_MODEL ** -0.5),
        "b_router": nrm(ks[17], (DEPTH, N_EXPERTS), 0.01),
        "w_gu": nrm(ks[18], (DEPTH, N_EXPERTS, D_MODEL, 2 * D_EXPERT), D_MODEL ** -0.5),
        "b_gu": nrm(ks[19], (DEPTH, N_EXPERTS, 2 * D_EXPERT), 0.02),
        "w_down": nrm(ks[20], (DEPTH, N_EXPERTS, D_EXPERT, D_MODEL), D_EXPERT ** -0.5 * DEEPNORM_BETA),
        "b_down": nrm(ks[21], (DEPTH, N_EXPERTS, D_MODEL), 0.02),
        "ln3_g": 1.0 + nrm(ks[22], (DEPTH, D_MODEL), 0.05),
        "ln3_b": nrm(ks[23], (DEPTH, D_MODEL), 0.02),
    }


def reference(x_prompt, x_sample, mem_prompt, mem_sample, w_in, w_pool, pool_scale, rpb, w_out,
              ln1_g, ln1_b, w_xq, w_xkv, w_xo, ln2_g, ln2_b,
              w_router, b_router, w_gu, b_gu, w_down, b_down, ln3_g, ln3_b):
    def run(x, mem):
        for l in range(DEPTH):
            x = encoder_layer(x, mem, w_in[l], w_pool[l], pool_scale[l], rpb[l], w_out[l],
                              ln1_g[l], ln1_b[l], w_xq[l], w_xkv[l], w_xo[l], ln2_g[l], ln2_b[l],
                              w_router[l], b_router[l], w_gu[l], b_gu[l], w_down[l], b_down[l],
                              ln3_g[l], ln3_b[l])
        return x

    y_prompt = run(x_prompt, mem_prompt)
    y_sample = run(x_sample, mem_sample)
    return (y_prompt, y_sample)
```

```python
from contextlib import ExitStack
import numpy as np
import concourse.bass as bass
import concourse.mybir as mybir
from concourse.bass_utils import run_bass_kernel_spmd

F32 = mybir.dt.float32
BF16 = mybir.dt.bfloat16
I32 = mybir.dt.int32
AF = mybir.ActivationFunctionType
ALU = mybir.AluOpType

D = 2048
NSLAB = 4096
NOWN = 3072
NBLK = 6
NE = 32
CAP = 512
NSLOT = NE * CAP
ALPHA = 2.0 ** 0.25
EPS = 1e-5
ENGS = ("pe", "act", "dve", "pool", "sp")


class Res:
    __slots__ = ("name", "last_w", "readers", "sem", "cnt")

    def __init__(self, name):
        self.name = name
        self.last_w = None
        self.readers = []
        self.sem = None
        self.cnt = 0


class Op:
    __slots__ = ("eng", "emit", "deps", "dma_res", "n_dma", "tok_sem", "tok_val", "needed")


class Prog:
    def __init__(self, nc, tag):
        self.nc = nc
        self.tag = tag
        self.ops = []
        self.by_eng = {e: [] for e in ENGS}

    def op(self, eng, emit, reads=(), writes=(), dma_res=None, n_dma=0):
        o = Op()
        o.eng, o.emit, o.dma_res, o.n_dma, o.needed = eng, emit, dma_res, n_dma, False
        o.tok_val = None
        deps = set()
        for r in reads:
            if r.last_w is not None:
                deps.add(r.last_w)
        for w in writes:
            if w.last_w is not None:
                deps.add(w.last_w)
            deps.update(w.readers)
        for r in reads:
            r.readers.append(o)
        for w in writes:
            w.last_w = o
            w.readers = []
        deps.discard(o)
        if eng == "pe" and dma_res is None:
            deps = {d for d in deps if not (d.eng == "pe" and d.dma_res is None)}
        o.deps = deps
        for d in deps:
            d.needed = True
        if dma_res is not None:
            dma_res.cnt += 16 * n_dma
            o.tok_val = dma_res.cnt
        self.ops.append(o)
        self.by_eng[eng].append(o)
        return o

    def barrier(self):
        last = []
        for e in ENGS:
            for o in reversed(self.by_eng[e]):
                if o.dma_res is None and o.emit is not None:
                    last.append(o)
                    break
        dmas = {}
        for o in self.ops:
            if o.dma_res is not None:
                dmas[id(o.dma_res)] = o
        alld = last + list(dmas.values())
        for d in alld:
            d.needed = True
        for e in ENGS:
            o = Op()
            o.eng, o.emit, o.dma_res, o.n_dma, o.needed = e, None, None, 0, False
            o.tok_val = None
            o.deps = set(alld)
            self.ops.append(o)
            self.by_eng[e].append(o)

    def assign(self, sems):
        self.eng_sem = {e: sems(f"{self.tag}_e_{e}") for e in ENGS}
        ms = {e: 0 for e in ENGS}
        for o in self.ops:
            if o.dma_res is not None:
                if o.dma_res.sem is None:
                    o.dma_res.sem = sems(f"{self.tag}_d_{o.dma_res.name}")
                o.tok_sem = o.dma_res.sem
            else:
                o.tok_sem = self.eng_sem[o.eng]
                if o.needed:
                    ms[o.eng] += 1
                    o.tok_val = ms[o.eng]

    def emit(self, block):
        def run_engine(e, eng):
            known = {}
            for o in self.by_eng[e]:
                waits = {}
                for d in o.deps:
                    k = id(d.tok_sem)
                    v = d.tok_val
                    if known.get(k, 0) >= v:
                        continue
                    if k not in waits or waits[k][1] < v:
                        waits[k] = (d.tok_sem, v)
                for k, (s, v) in waits.items():
                    eng.wait_ge(s, v)
                    known[k] = v
                if o.emit is None:
                    continue
                instrs = o.emit(eng)
                if o.dma_res is not None:
                    assert len(instrs) == o.n_dma, (len(instrs), o.n_dma)
                    for ins in instrs:
                        ins.then_inc(o.tok_sem, 16)
                elif o.needed:
                    instrs[-1].then_inc(o.tok_sem, 1)

        @block.tensor
        def _(eng):
            run_engine("pe", eng)

        @block.scalar
        def _(eng):
            run_engine("act", eng)

        @block.vector
        def _(eng):
            run_engine("dve", eng)

        @block.gpsimd
        def _(eng):
            run_engine("pool", eng)

        @block.sync
        def _(eng):
            run_engine("sp", eng)


class Stage:
    def __init__(self, nc, tag):
        self.nc, self.tag = nc, tag
        self.stack = ExitStack()
        self.P = Prog(nc, tag)
        self.nps = 0
        self.rr = 0

    def sb(self, name, shape, dt):
        return self.stack.enter_context(self.nc.sbuf_tensor(f"{self.tag}_{name}", shape, dt))

    def ps(self, dt=F32, cols=512):
        self.nps += 1
        assert self.nps <= 8
        return self.stack.enter_context(self.nc.psum_tensor(f"{self.tag}_ps{self.nps}", [128, cols], dt))

    def finish(self):
        self.P.barrier()
        hs = []

        def mk(n):
            hs.append(self.nc.alloc_semaphore(n))
            return hs[-1]
        self.P.assign(mk)
        if _SUB.get("scopes"):
            with self.nc.named_scope(self.tag):
                with self.nc.Block() as block:
                    self.P.emit(block)
        else:
            with self.nc.Block() as block:
                self.P.emit(block)
        self.nc.clear_and_free_semaphores(hs)
        self.nc.all_engine_barrier()
        self.stack.close()

    def dma(self, out, in_, reads=(), writes=(), res=None, eng="sp"):
        self.P.op(eng, lambda e: [e.dma_start(out=out, in_=in_)], reads=reads, writes=writes, dma_res=res, n_dma=1)

    def evac(self, out, in_, reads, writes, scale=None):
        self.rr += 1
        if self.rr % 2 == 0:
            if scale is None:
                self.P.op("act", lambda e: [e.activation(out=out, in_=in_, func=AF.Copy)], reads=reads, writes=writes)
            else:
                self.P.op("act", lambda e: [e.activation(out=out, in_=in_, func=AF.Copy, scale=scale)], reads=reads, writes=writes)
        else:
            if scale is None:
                self.P.op("dve", lambda e: [e.tensor_copy(out=out, in_=in_)], reads=reads, writes=writes)
            else:
                self.P.op("dve", lambda e: [e.tensor_scalar(out=out, in0=in_, scalar1=float(scale), scalar2=None, op0=ALU.mult)],
                          reads=reads, writes=writes)


def load_w_bf16(st, wsb, w_view, r_w, nk_split=4):
    step = 16 // nk_split
    st.P.op("pool", lambda e: [e.dma_start(out=wsb[:, i * step:(i + 1) * step, :], in_=w_view[:, i * step:(i + 1) * step, :])
                               for i in range(nk_split)], writes=[r_w], dma_res=r_w, n_dma=nk_split)


def stage_proj(nc, tag, src, ntok, w, c0, wcols, fm_outs, tm_outs, ident_d):
    st = Stage(nc, tag)
    P = st.P
    wsb = st.sb("w", [128, 16, wcols], BF16)
    ident = st.sb("ident", [128, 128], F32)
    xt = st.sb("xt", [128, 4, D], F32)
    xT = st.sb("xT", [128, 2, 16, 512], BF16)
    ost = st.sb("ost", [128, 4, 512], BF16)
    vst = st.sb("vst", [128, 2, 512], BF16)
    pst = [st.ps() for _ in range(2)]
    psm = [st.ps() for _ in range(6)]
    r_w, r_id = Res("w"), Res("id")
    r_xt = [Res(f"xt{t}") for t in range(4)]
    r_xT = [[Res(f"xT{p}_{k}") for k in range(16)] for p in range(2)]
    r_ost = [Res(f"ost{i}") for i in range(4)]
    r_vst = [Res(f"vst{i}") for i in range(2)]
    r_pst = [Res(f"pst{i}") for i in range(2)]
    r_psm = [Res(f"psm{i}") for i in range(6)]
    st.dma(ident[:], ident_d, writes=[r_id], res=r_id)
    load_w_bf16(st, wsb, w.rearrange("(k p) n -> p k n", p=128)[:, :, c0:c0 + wcols], r_w)
    nblk = ntok // 512
    mi = 0
    oi = 0
    vi = 0
    def xt_loads(blk):
        for t in range(4):
            r0 = blk * 512 + t * 128
            st.dma(xt[:, t, :], src[r0:r0 + 128, :], writes=[r_xt[t]], res=r_xt[t])
    xt_loads(0)
    for blk in range(nblk):
        par = blk % 2
        for k in range(16):
            b = k % 2
            P.op("pe", lambda e, k=k, b=b: [e.transpose(out=pst[b][:, t * 128:(t + 1) * 128], in_=xt[:, t, k * 128:(k + 1) * 128],
                                                       identity=ident[:]) for t in range(4)],
                 reads=[r_id] + r_xt, writes=[r_pst[b]])
            st.evac(xT[:, par, k, :], pst[b][:, :], reads=[r_pst[b]], writes=[r_xT[par][k]])
        if blk + 1 < nblk:
            xt_loads(blk + 1)
        for (coff, nch, dst, scale) in fm_outs:
            for oc in range(nch):
                b = mi % 6
                mi += 1
                P.op("pe", lambda e, oc=oc, b=b, coff=coff, par=par: [
                    e.matmul(psm[b][:, :], lhsT=wsb[:, k, coff + oc * 128:coff + (oc + 1) * 128], rhs=xT[:, par, k, :],
                             start=(k == 0), stop=(k == 15)) for k in range(16)],
                     reads=[r_w] + r_xT[par], writes=[r_psm[b]])
                o = oi % 4
                oi += 1
                st.evac(ost[:, o, :], psm[b][:, :], reads=[r_psm[b]], writes=[r_ost[o]], scale=scale)
                st.dma(dst[oc, :, blk * 512:(blk + 1) * 512], ost[:, o, :], reads=[r_ost[o]], res=r_ost[o])
        for (coff, ncols, dst) in tm_outs:
            for t in range(4):
                for n in range(ncols // 512):
                    b = mi % 6
                    mi += 1
                    P.op("pe", lambda e, t=t, n=n, b=b, coff=coff, par=par: [
                        e.matmul(psm[b][:, :], lhsT=xT[:, par, k, t * 128:(t + 1) * 128],
                                 rhs=wsb[:, k, coff + n * 512:coff + (n + 1) * 512], start=(k == 0), stop=(k == 15))
                        for k in range(16)], reads=[r_w] + r_xT[par], writes=[r_psm[b]])
                    o = vi % 2
                    vi += 1
                    st.evac(vst[:, o, :], psm[b][:, :], reads=[r_psm[b]], writes=[r_vst[o]])
                    r0 = blk * 512 + t * 128
                    st.dma(dst[r0:r0 + 128, n * 512:(n + 1) * 512], vst[:, o, :], reads=[r_vst[o]], res=r_vst[o])
    st.finish()


def stage_proj_ln(nc, tag, actT_d, w, resid, gam_d, bet_d, out32, out16=None):
    st = Stage(nc, tag)
    P = st.P
    NB = 3
    wsb = st.sb("w", [128, 16, D], BF16)
    gam = st.sb("gam", [128, D], F32)
    bet = st.sb("bet", [128, D], F32)
    aT = st.sb("aT", [128, 2, 16, 512], BF16)
    xr = st.sb("xr", [128, NB, D], F32)
    y = st.sb("y", [128, NB, D], F32)
    y16 = st.sb("y16", [128, 2, D], BF16)
    stats = st.sb("stats", [128, NB, 4, 6], F32)
    mv = st.sb("mv", [128, NB, 4], F32)
    ps = [st.ps() for _ in range(8)]
    r_w, r_g, r_b = Res("w"), Res("g"), Res("b")
    r_aT = [Res(f"aT{i}") for i in range(2)]
    r_xr = [Res(f"xr{i}") for i in range(NB)]
    r_y = [Res(f"y{i}") for i in range(NB)]
    r_y16 = [Res(f"y16{i}") for i in range(2)]
    r_st = [Res(f"st{i}") for i in range(NB)]
    r_ps = [Res(f"ps{i}") for i in range(8)]
    load_w_bf16(st, wsb, w.rearrange("(k p) n -> p k n", p=128), r_w)
    st.dma(gam[:], gam_d, writes=[r_g], res=r_g)
    st.dma(bet[:], bet_d, writes=[r_b], res=r_b)
    ti = 0
    for blk in range(NBLK):
        par = blk % 2
        st.dma(aT[:, par, :, :], actT_d.rearrange("c p n -> p c n")[:, :, blk * 512:(blk + 1) * 512], writes=[r_aT[par]], res=r_aT[par])
        for t in range(4):
            q = ti % NB
            pq = ti % 2
            h16 = ti % 2
            ti += 1
            r0 = blk * 512 + t * 128
            st.dma(xr[:, q, :], resid[r0:r0 + 128, :], writes=[r_xr[q]], res=r_xr[q])
            for n in range(4):
                b = pq * 4 + n
                P.op("pe", lambda e, t=t, n=n, b=b, par=par: [
                    e.matmul(ps[b][:, :], lhsT=aT[:, par, k, t * 128:(t + 1) * 128], rhs=wsb[:, k, n * 512:(n + 1) * 512],
                             start=(k == 0), stop=(k == 15)) for k in range(16)], reads=[r_w, r_aT[par]], writes=[r_ps[b]])
                P.op("dve", lambda e, n=n, b=b, q=q: [
                    e.scalar_tensor_tensor(out=y[:, q, n * 512:(n + 1) * 512], in0=xr[:, q, n * 512:(n + 1) * 512], scalar=ALPHA,
                                           in1=ps[b][:, :], op0=ALU.mult, op1=ALU.add)],
                     reads=[r_xr[q], r_ps[b]], writes=[r_y[q]])
            ln_tail(st, y[:, q, :], stats[:, q, :, :], mv[:, q, :], gam, bet, r_y[q], r_st[q], [r_g, r_b])
            st.dma(out32[r0:r0 + 128, :], y[:, q, :], reads=[r_y[q]], res=r_y[q], eng="pool")
            if out16 is not None:
                P.op("act", lambda e, q=q, h16=h16: [e.activation(out=y16[:, h16, :], in_=y[:, q, :], func=AF.Copy)], reads=[r_y[q]], writes=[r_y16[h16]])
                st.dma(out16[r0:r0 + 128, :], y16[:, h16, :], reads=[r_y16[h16]], res=r_y16[h16], eng="pool")
    st.finish()


def ln_tail(st, yv, statv, mvv, gam, bet, r_y, r_st, r_gb):
    P = st.P
    for n in range(4):
        P.op("dve", lambda e, n=n: [e.bn_stats(out=statv[:, n, :], in_=yv[:, n * 512:(n + 1) * 512])], reads=[r_y], writes=[r_st])
    P.op("dve", lambda e: [e.bn_aggr(out=mvv[:, 0:2], in_=statv)], reads=[r_st], writes=[r_st])
    P.op("act", lambda e: [e.activation(out=mvv[:, 2:3], in_=mvv[:, 1:2], func=AF.Sqrt, bias=EPS, scale=1.0)], reads=[r_st], writes=[r_st])
    P.op("dve", lambda e: [e.reciprocal(out=mvv[:, 2:3], in_=mvv[:, 2:3])], reads=[r_st], writes=[r_st])
    P.op("dve", lambda e: [e.scalar_tensor_tensor(out=mvv[:, 3:4], in0=mvv[:, 0:1], scalar=-1.0, in1=mvv[:, 2:3],
                                                  op0=ALU.mult, op1=ALU.mult)], reads=[r_st], writes=[r_st])
    P.op("act", lambda e: [e.activation(out=yv, in_=yv, func=AF.Identity, bias=mvv[:, 3:4], scale=mvv[:, 2:3])],
         reads=[r_st, r_y], writes=[r_y])
    P.op("dve", lambda e: [e.tensor_tensor(out=yv, in0=yv, in1=gam[:], op=ALU.mult)], reads=[r_y, r_gb[0]], writes=[r_y])
    P.op("pool", lambda e: [e.tensor_tensor(out=yv, in0=yv, in1=bet[:], op=ALU.add)], reads=[r_y, r_gb[1]], writes=[r_y])


def ks_of(b):
    return 4 * b if b < 8 else 4 * b + 8


def stage_mix(nc, AT_d, QT_d, KT_d, V_d, mix_d, w_pool, pscale_d, g2_d, rm_d, e2_d, invc_d, ident_d):
    st = Stage(nc, "s2a")
    P = st.P
    wp = st.sb("wp", [128, 4, 2, 256], BF16)
    psc = st.sb("psc", [128, 8], F32)
    g2f = st.sb("g2f", [128, 2, 14 * 64], F32)
    g2 = st.sb("g2", [128, 16, 14 * 64], BF16)
    idf = st.sb("idf", [128, 128], F32)
    idb = st.sb("idb", [128, 128], BF16)
    e2f = st.sb("e2f", [2, 128], F32)
    e2 = st.sb("e2", [2, 128], BF16)
    rmf = st.sb("rmf", [2, 288], F32)
    rm = st.sb("rm", [2, 288], BF16)
    ones = st.sb("ones", [128, 64], BF16)
    aT = st.sb("aT", [128, 8, 544], BF16)
    invc = st.sb("invc", [128, 4, 512], F32)
    T = st.sb("T", [128, 4, 544], F32)
    pT = st.sb("pT", [128, 8, 512], BF16)
    mixT = st.sb("mixT", [128, 2, 16, 512], BF16)
    KT = st.sb("KT", [128, 2, 1024], BF16)
    QT = st.sb("QT", [128, 2, 512], BF16)
    V = st.sb("V", [128, 2, 8, 128], BF16)
    PT = st.sb("PT", [128, 4, 512], BF16)
    rec = st.sb("rec", [128, 2, 256], F32)
    ps_s = [st.ps() for _ in range(3)]
    ps_o = [st.ps() for _ in range(2)]
    ps_d = [st.ps() for _ in range(2)]
    ps_p = st.ps()
    R = lambda n: Res(n)
    r_wp, r_psc, r_g2f, r_g2, r_idf, r_idb, r_e2f, r_e2, r_rmf, r_rm, r_ones = (R(n) for n in
        ("wp", "psc", "g2f", "g2", "idf", "idb", "e2f", "e2", "rmf", "rm", "ones"))
    r_aT, r_invc, r_T = R("aT"), R("invc"), R("T")
    r_mix = [R("mixT0"), R("mixT1")]
    r_pT = [R(f"pT{c}") for c in range(8)]
    r_hp = [R(f"hp{i}") for i in range(2)]
    r_PT = [R(f"PT{i}") for i in range(4)]
    r_rec = [R(f"rec{i}") for i in range(2)]
    r_pss = [R(f"pss{i}") for i in range(3)]
    r_pso = [R(f"pso{i}") for i in range(2)]
    r_psd = [R(f"psd{i}") for i in range(2)]
    r_psp = R("psp")
    P.op("pool", lambda e: [e.dma_start(out=wp[:], in_=w_pool.rearrange("g (cc p) d -> p g cc d", p=128))], writes=[r_wp], dma_res=r_wp, n_dma=1)
    st.dma(psc[:], pscale_d, writes=[r_psc], res=r_psc)
    st.dma(idf[:], ident_d, writes=[r_idf], res=r_idf)
    st.dma(e2f[:], e2_d, writes=[r_e2f], res=r_e2f)
    st.dma(rmf[:], rm_d, writes=[r_rmf], res=r_rmf)
    P.op("dve", lambda e: [e.tensor_copy(out=idb[:], in_=idf[:])], reads=[r_idf], writes=[r_idb])
    P.op("dve", lambda e: [e.tensor_copy(out=e2[:], in_=e2f[:])], reads=[r_e2f], writes=[r_e2])
    P.op("dve", lambda e: [e.tensor_copy(out=rm[:], in_=rmf[:])], reads=[r_rmf], writes=[r_rm])
    P.op("dve", lambda e: [e.memset(ones[:], 1.0)], writes=[r_ones])
    for h2 in range(8):
        st.dma(g2f[:, :, :], g2_d[:, 2 * h2:2 * h2 + 2, :], writes=[r_g2f], res=r_g2f)
        P.op("dve", lambda e, h2=h2: [e.tensor_copy(out=g2[:, 2 * h2:2 * h2 + 2, :], in_=g2f[:, :, :])], reads=[r_g2f], writes=[r_g2])
    si = 0
    oi = 0
    pi = 0
    its = []
    pool_parts = {}
    for B in range(NBLK):
        b0 = 2 * B
        ks0 = ks_of(b0)
        st0 = (ks0 + 4) * 64
        mb = B % 2

        def pool_part(B=B, st0=st0, mb=mb):
            if B >= NBLK:
                return
            st.dma(aT[:, :, :], AT_d.rearrange("c p n -> p c n")[:, :, st0 - 16:st0 + 528], writes=[r_aT], res=r_aT)
            st.dma(invc[:, :, :], invc_d[:, B * 512:(B + 1) * 512].partition_broadcast(128), writes=[r_invc], res=r_invc)
            for c in range(8):
                g = c // 2

                def pool_ops(e, c=c, g=g):
                    ins = [e.tensor_tensor(out=T[:, 0, 1:544], in0=aT[:, c, 0:543], in1=aT[:, c, 1:544], op=ALU.add)]
                    if g >= 1:
                        ins.append(e.tensor_tensor(out=T[:, 1, 2:543], in0=T[:, 0, 1:542], in1=T[:, 0, 3:544], op=ALU.add))
                    if g >= 2:
                        ins.append(e.tensor_tensor(out=T[:, 2, 4:541], in0=T[:, 1, 2:539], in1=T[:, 1, 6:543], op=ALU.add))
                    if g >= 3:
                        ins.append(e.tensor_tensor(out=T[:, 3, 8:537], in0=T[:, 2, 4:533], in1=T[:, 2, 12:541], op=ALU.add))
                    ins.append(e.tensor_tensor(out=T[:, g, 16:528], in0=T[:, g, 16:528], in1=invc[:, g, :], op=ALU.mult))
                    ins.append(e.tensor_tensor(out=pT[:, c, :], in0=T[:, g, 16:528], in1=aT[:, c, 16:528], op=ALU.subtract))
                    return ins
                n_ins = 3 + g
                for ii in range(n_ins):
                    P.op("pool", lambda e, c=c, g=g, ii=ii, f=pool_ops: [_nth(f, e, ii)], reads=[r_aT, r_invc, r_T],
                         writes=[r_T] + ([r_pT[c]] if ii == n_ins - 1 else []))
            for dc in range(8):
                g, dh = dc // 2, dc % 2
                P.op("pe", lambda e, g=g, dh=dh: [e.matmul(ps_p[:, :], lhsT=wp[:, g, cc, dh * 128:(dh + 1) * 128], rhs=pT[:, 2 * g + cc, :],
                                                            start=(cc == 0), stop=(cc == 1)) for cc in range(2)],
                     reads=[r_wp, r_pT[2 * g], r_pT[2 * g + 1]], writes=[r_psp])
                P.op("act", lambda e, dc=dc, mb=mb: [e.activation(out=mixT[:, mb, dc, :], in_=ps_p[:, :], func=AF.Copy, scale=psc[:, dc:dc + 1])],
                     reads=[r_psp, r_psc], writes=[r_mix[mb]])
        pool_parts[B] = pool_part
        for hp in range(8):
            q = hp % 2

            def load_hp(hp=hp, q=q, ks0=ks0, st0=st0):
                P.op("sp", lambda e: [
                    e.dma_start(out=KT[:, q, :], in_=KT_d[hp, :, ks0 * 64:ks0 * 64 + 1024]),
                    e.dma_start(out=QT[:, q, :], in_=QT_d[hp, :, st0:st0 + 512]),
                    e.dma_start(out=V[:, q, :, :], in_=V_d[ks0 * 64:ks0 * 64 + 1024, hp * 128:(hp + 1) * 128].rearrange("(t p) f -> p t f", p=128))],
                     writes=[r_hp[q]], dma_res=r_hp[q], n_dma=3)
            for bi in range(2):
                b = b0 + bi
                o = oi % 2
                oi += 1
                for pr in range(6):
                    s = si % 3
                    si += 1
                    koff = bi * 256 + pr * 128
                    vt = bi * 2 + pr
                    p = pi % 4
                    pi += 1
                    pre = []
                    if B == 0 and hp == 0 and bi == 0 and pr == 0:
                        pre.append(lambda: pool_parts[0]())
                    if hp == 4 and bi == 0 and pr == 0 and B + 1 < NBLK:
                        pre.append(lambda B=B: pool_parts[B + 1]())
                    if bi == 0 and pr == 0:
                        pre.append(load_hp)

                    def front(hp=hp, q=q, bi=bi, b=b, pr=pr, s=s, koff=koff, p=p):
                        def smm(e):
                            ins = []
                            for hh in range(2):
                                lo, hi = hh * 64, hh * 64 + 64
                                h = 2 * hp + hh
                                out = ps_s[s][:, hh * 256:(hh + 1) * 256]
                                ins.append(e.matmul(out, lhsT=KT[lo:hi, q, koff:koff + 128], rhs=QT[lo:hi, q, bi * 256:(bi + 1) * 256],
                                                    start=True, stop=False))
                                ins.append(e.matmul(out, lhsT=idb[:, :], rhs=g2[:, h, (10 - 2 * pr) * 64:(10 - 2 * pr) * 64 + 256],
                                                    start=False, stop=False))
                                roff = (b * 6 + pr) * 4
                                ins.append(e.matmul(out, lhsT=e2[0:2, :], rhs=rm[0:2, roff:roff + 4].unsqueeze(2).broadcast_to([2, 4, 64]),
                                                    start=False, stop=True))
                            return ins
                        P.op("pe", smm, reads=[r_hp[q], r_idb, r_g2, r_e2, r_rm], writes=[r_pss[s]])
                        P.op("act", lambda e: [e.activation(out=PT[:, p, :], in_=ps_s[s][:, :], func=AF.Exp)],
                             reads=[r_pss[s]], writes=[r_PT[p]])

                    def back(hp=hp, q=q, bi=bi, vt=vt, p=p, o=o, pr=pr, mb=mb, B=B):
                        def pvmm(e):
                            ins = []
                            for hh in range(2):
                                lo, hi = hh * 64, hh * 64 + 64
                                ins.append(e.matmul(ps_o[o][lo:hi, 0:256], lhsT=V[:, q, vt, lo:hi], rhs=PT[:, p, hh * 256:(hh + 1) * 256],
                                                    start=(pr == 0), stop=(pr == 5)))
                                ins.append(e.matmul(ps_d[o][lo:hi, 0:256], lhsT=ones[:, 0:64], rhs=PT[:, p, hh * 256:(hh + 1) * 256],
                                                    start=(pr == 0), stop=(pr == 5)))
                            return ins
                        P.op("pe", pvmm, reads=[r_hp[q], r_PT[p], r_ones], writes=[r_pso[o], r_psd[o]])
                        if pr == 5:
                            P.op("dve", lambda e: [e.reciprocal(out=rec[:, o, :], in_=ps_d[o][:, 0:256])], reads=[r_psd[o]], writes=[r_rec[o]])
                            P.op("dve", lambda e: [e.tensor_tensor(out=mixT[:, mb, 8 + hp, bi * 256:(bi + 1) * 256], in0=ps_o[o][:, 0:256],
                                                                   in1=rec[:, o, :], op=ALU.mult)],
                                 reads=[r_pso[o], r_rec[o]], writes=[r_mix[mb]])
                            if hp == 7 and bi == 1:
                                st.dma(mix_d.rearrange("c p n -> p c n")[:, :, B * 512:(B + 1) * 512], mixT[:, mb, :, :], reads=[r_mix[mb]], res=r_mix[mb])
                    its.append((pre, front, back))
    SK = 2
    for idx in range(len(its) + SK):
        if idx < len(its):
            for f in its[idx][0]:
                f()
            its[idx][1]()
        if idx >= SK:
            its[idx - SK][2]()
    st.finish()


def _nth(f, e, ii):
    return _NthEngine(e, ii).run(f)


class _NthEngine:
    def __init__(self, e, ii):
        self.e, self.ii, self.n, self.res = e, ii, 0, None

    def __getattr__(self, name):
        def call(*a, **k):
            i = self.n
            self.n += 1
            if i == self.ii:
                self.res = getattr(self.e, name)(*a, **k)
                return self.res
            return None
        return call

    def run(self, f):
        f(self)
        return self.res


def stage_xattn(nc, QX_d, KTm_d, Vm_d, oT_d):
    st = Stage(nc, "s3c")
    P = st.P
    KTm = st.sb("KTm", [128, 16, 512], BF16)
    Vm = st.sb("Vm", [128, 4, D], BF16)
    ones = st.sb("ones", [128, 128], BF16)
    QX = st.sb("QX", [128, 2, 16, 512], BF16)
    PT = st.sb("PT", [128, 4, 512], BF16)
    rec = st.sb("rec", [128, 2, 512], F32)
    oT = st.sb("oT", [128, 2, 16, 512], BF16)
    ps_s = [st.ps() for _ in range(3)]
    ps_d = [st.ps() for _ in range(2)]
    ps_o = [st.ps() for _ in range(3)]
    r_k, r_v, r_ones = Res("k"), Res("v"), Res("ones")
    r_qx = [Res(f"qx{i}") for i in range(2)]
    r_oT = [Res(f"oT{i}") for i in range(2)]
    r_PT = [Res(f"PT{i}") for i in range(4)]
    r_rec = [Res(f"rec{i}") for i in range(2)]
    r_pss = [Res(f"pss{i}") for i in range(3)]
    r_psd = [Res(f"psd{i}") for i in range(2)]
    r_pso = [Res(f"pso{i}") for i in range(3)]
    st.dma(KTm[:, :, :], KTm_d.rearrange("c p n -> p c n"), writes=[r_k], res=r_k)
    st.dma(Vm[:, :, :], Vm_d.rearrange("(t p) f -> p t f", p=128), writes=[r_v], res=r_v)
    P.op("dve", lambda e: [e.memset(ones[:], 1.0)], writes=[r_ones])
    si = pi = di = oi = 0
    its = []
    for B in range(NBLK):
        par = B % 2
        seq = 0 if B < 4 else 1
        for h in range(4):
            ss, pts = [], []
            for mc in range(2):
                ss.append(si % 3)
                si += 1
                pts.append(pi % 4)
                pi += 1
            d = di % 2
            di += 1
            os_ = []
            for dk in range(4):
                os_.append(oi % 3)
                oi += 1

            def front(B=B, par=par, seq=seq, h=h, ss=tuple(ss), pts=tuple(pts)):
                if h == 0:
                    st.dma(QX[:, par, :, :], QX_d.rearrange("c p n -> p c n")[:, :, B * 512:(B + 1) * 512], writes=[r_qx[par]], res=r_qx[par])
                for mc in range(2):
                    s, p = ss[mc], pts[mc]
                    P.op("pe", lambda e, mc=mc, s=s: [
                        e.matmul(ps_s[s][:, :], lhsT=KTm[:, 4 * h + dk, seq * 256 + mc * 128:seq * 256 + (mc + 1) * 128],
                                 rhs=QX[:, par, 4 * h + dk, :], start=(dk == 0), stop=(dk == 3)) for dk in range(4)],
                         reads=[r_k, r_qx[par]], writes=[r_pss[s]])
                    P.op("act", lambda e, s=s, p=p: [e.activation(out=PT[:, p, :], in_=ps_s[s][:, :], func=AF.Exp)],
                         reads=[r_pss[s]], writes=[r_PT[p]])

            def back(B=B, par=par, seq=seq, h=h, pts=tuple(pts), d=d, os_=tuple(os_)):
                P.op("pe", lambda e: [e.matmul(ps_d[d][:, :], lhsT=ones[:, :], rhs=PT[:, pts[mc], :], start=(mc == 0), stop=(mc == 1))
                                      for mc in range(2)], reads=[r_ones, r_PT[pts[0]], r_PT[pts[1]]], writes=[r_psd[d]])
                P.op("dve", lambda e: [e.reciprocal(out=rec[:, d, :], in_=ps_d[d][:, :])], reads=[r_psd[d]], writes=[r_rec[d]])
                for dk in range(4):
                    o = os_[dk]
                    P.op("pe", lambda e, o=o, dk=dk: [
                        e.matmul(ps_o[o][:, :], lhsT=Vm[:, seq * 2 + mc, (4 * h + dk) * 128:(4 * h + dk + 1) * 128], rhs=PT[:, pts[mc], :],
                                 start=(mc == 0), stop=(mc == 1)) for mc in range(2)],
                         reads=[r_v, r_PT[pts[0]], r_PT[pts[1]]], writes=[r_pso[o]])
                    P.op("dve", lambda e, o=o, dk=dk: [e.tensor_tensor(out=oT[:, par, 4 * h + dk, :], in0=ps_o[o][:, :],
                                                                       in1=rec[:, d, :], op=ALU.mult)],
                         reads=[r_pso[o], r_rec[d]], writes=[r_oT[par]])
                if h == 3:
                    st.dma(oT_d.rearrange("c p n -> p c n")[:, :, B * 512:(B + 1) * 512], oT[:, par, :, :], reads=[r_oT[par]], res=r_oT[par])
            its.append((front, back))
    SK = 1
    for idx in range(len(its) + SK):
        if idx < len(its):
            its[idx][0]()
        if idx >= SK:
            its[idx - SK][1]()
    st.finish()


def stage_router(nc, x2_d, wr_d, brep_d, ident_d, triu_d, iotaE_d, tokid_d, tab_d, tabinit_d, sidx_d, GT_d, yzero_d, Y_d, x2b_d):
    st = Stage(nc, "s4")
    P = st.P
    wr = st.sb("wr", [128, 16, NE], F32)
    brep = st.sb("brep", [128, NE], F32)
    idf = st.sb("idf", [128, 128], F32)
    triu = st.sb("triu", [128, 128], F32)
    triub = st.sb("triub", [128, 128], BF16)
    onesb = st.sb("onesb", [128, 128], BF16)
    iotaE = st.sb("iotaE", [128, NE], F32)
    tokid = st.sb("tokid", [128, 24], F32)
    carry = st.sb("carry", [128, NE], F32)
    tinit = st.sb("tinit", [128, NSLOT // 128 + 1, 2], F32)
    zrow = st.sb("zrow", [1, D], BF16)
    xt = st.sb("xt", [128, 2, D], F32)
    xT = st.sb("xT", [128, 2, 16, 128], F32)
    lg = st.sb("lg", [128, 2, 8, NE], F32)
    Ab = st.sb("Ab", [128, 2, NE], BF16)
    m8 = st.sb("m8", [128, 2, 32], F32)
    si4 = st.sb("si4", [128, 2, 4], I32)
    sc = st.sb("sc", [128, 2, 4, 2], F32)
    GT = st.sb("GT", [32, 2, 128], F32)
    ps_t = [st.ps() for _ in range(2)]
    ps_l = [st.ps() for _ in range(2)]
    ps_p = [st.ps() for _ in range(2)]
    ps_g = [st.ps() for _ in range(2)]
    R = lambda n: Res(n)
    r_wr, r_brep, r_idf, r_triu, r_triub, r_onesb, r_iotaE, r_tokid, r_carry, r_tinit, r_tab, r_zrow = (R(n) for n in (
        "wr", "brep", "idf", "triu", "triub", "onesb", "iotaE", "tokid", "carry", "tinit", "tab", "zrow"))
    r_xt = [R(f"xt{i}") for i in range(2)]
    r_xT = [R(f"xT{i}") for i in range(2)]
    r_w = [R(f"w{i}") for i in range(2)]
    r_sc = [R(f"sc{i}") for i in range(2)]
    r_si = [R(f"si{i}") for i in range(2)]
    r_GT = [R(f"GT{i}") for i in range(2)]
    r_pst = [R(f"pst{i}") for i in range(2)]
    r_psl = [R(f"psl{i}") for i in range(2)]
    r_psp = [R(f"psp{i}") for i in range(2)]
    r_psg = [R(f"psg{i}") for i in range(2)]
    st.dma(wr[:, :, :], wr_d.rearrange("(k p) n -> p k n", p=128), writes=[r_wr], res=r_wr)
    st.dma(brep[:], brep_d, writes=[r_brep], res=r_brep)
    st.dma(idf[:], ident_d, writes=[r_idf], res=r_idf)
    st.dma(triu[:], triu_d, writes=[r_triu], res=r_triu)
    st.dma(iotaE[:], iotaE_d, writes=[r_iotaE], res=r_iotaE)
    st.dma(tokid[:], tokid_d, writes=[r_tokid], res=r_tokid)
    st.dma(tinit[:, :, :], tabinit_d, writes=[r_tinit], res=r_tinit)
    P.op("dve", lambda e: [e.tensor_copy(out=triub[:], in_=triu[:])], reads=[r_triu], writes=[r_triub])
    P.op("dve", lambda e: [e.memset(onesb[:], 1.0)], writes=[r_onesb])
    P.op("dve", lambda e: [e.memset(carry[:], 0.0)], writes=[r_carry])
    P.op("dve", lambda e: [e.memset(zrow[:], 0.0)], writes=[r_zrow])
    st.dma(tab_d[0:NSLOT, :].rearrange("(p t) c -> p t c", p=128), tinit[:, 0:NSLOT // 128, :], reads=[r_tinit], writes=[r_tab], res=r_tinit)
    st.dma(tab_d[NSLOT:NSLOT + 1, :], tinit[0:1, NSLOT // 128, :], reads=[r_tinit], writes=[r_tab], res=r_tinit)
    st.dma(Y_d[0:1, :], zrow[:, :], reads=[r_zrow], res=r_zrow)
    st.dma(x2b_d[NOWN:NOWN + 1, :], zrow[:, :], reads=[r_zrow], res=r_zrow)
    def part(ti, which):
        q = ti % 2
        L = lambda pl, q=q: lg[:, q, pl, :]
        M = lambda a, b2, q=q: m8[:, q, a:b2]
        if which == "B":
            return partB(ti, q, L, M)
        st.dma(xt[:, q, :], x2_d[ti * 128:(ti + 1) * 128, :], writes=[r_xt[q]], res=r_xt[q])
        for kk in range(4):
            b = kk % 2
            P.op("pe", lambda e, kk=kk, b=b, q=q: [e.transpose(out=ps_t[b][:, j * 128:(j + 1) * 128],
                                                               in_=xt[:, q, (4 * kk + j) * 128:(4 * kk + j + 1) * 128], identity=idf[:])
                                                   for j in range(4)], reads=[r_idf, r_xt[q]], writes=[r_pst[b]])
            st.evac(xT[:, q, 4 * kk:4 * kk + 4, :], ps_t[b][:, :].rearrange("p (j n) -> p j n", j=4), reads=[r_pst[b]], writes=[r_xT[q]])
        P.op("pe", lambda e, q=q: [e.matmul(ps_l[q][:, 0:NE], lhsT=xT[:, q, k, :], rhs=wr[:, k, :], start=(k == 0), stop=(k == 15))
                                   for k in range(16)], reads=[r_wr, r_xT[q]], writes=[r_psl[q]])
        W = [r_w[q]]

        def dv(f, reads=(), writes=None, q=q):
            P.op("dve", f, reads=list(reads) + [r_w[q]], writes=[r_w[q]] if writes is None else writes)
        dv(lambda e, q=q, L=L: [e.tensor_tensor(out=L(0), in0=ps_l[q][:, 0:NE], in1=brep[:], op=ALU.add)], reads=[r_psl[q], r_brep])
        dv(lambda e, L=L, M=M: [e.max(out=M(0, 8), in_=L(0))])
        dv(lambda e, L=L, M=M: [e.tensor_scalar(out=L(1), in0=L(0), scalar1=M(3, 4), scalar2=None, op0=ALU.is_ge)])
        dv(lambda e, M=M: [e.tensor_scalar(out=M(16, 17), in0=M(0, 1), scalar1=-1.0, scalar2=None, op0=ALU.mult)])
        P.op("act", lambda e, L=L, M=M: [e.activation(out=L(2), in_=L(0), func=AF.Exp, bias=M(16, 17), scale=1.0)], reads=W, writes=W)
        dv(lambda e, L=L, M=M: [e.tensor_tensor(out=L(3), in0=L(2), in1=L(1), op=ALU.mult)])
        dv(lambda e, L=L, M=M: [e.tensor_reduce(out=M(17, 18), in_=L(3), axis=mybir.AxisListType.X, op=ALU.add)])
        dv(lambda e, M=M: [e.reciprocal(out=M(18, 19), in_=M(17, 18))])
        dv(lambda e, L=L, M=M: [e.tensor_scalar(out=L(3), in0=L(3), scalar1=M(18, 19), scalar2=None, op0=ALU.mult)])
        dv(lambda e, L=L, q=q: [e.tensor_copy(out=Ab[:, q, :], in_=L(1))])

    def partB(ti, q, L, M):
        W = [r_w[q]]

        def dv(f, reads=(), writes=None, q=q):
            P.op("dve", f, reads=list(reads) + [r_w[q]], writes=[r_w[q]] if writes is None else writes)
        P.op("pe", lambda e, q=q: [e.matmul(ps_p[q][:, 0:NE], lhsT=triub[:, :], rhs=Ab[:, q, :], start=True, stop=True),
                                   e.matmul(ps_p[q][:, 64:64 + NE], lhsT=onesb[:, :], rhs=Ab[:, q, :], start=True, stop=True)],
             reads=[r_triub, r_onesb, r_w[q]], writes=[r_psp[q]])
        dv(lambda e, L=L, q=q: [e.tensor_tensor(out=L(4), in0=ps_p[q][:, 0:NE], in1=carry[:], op=ALU.add)], reads=[r_psp[q], r_carry])
        P.op("dve", lambda e, q=q: [e.tensor_tensor(out=carry[:], in0=carry[:], in1=ps_p[q][:, 64:64 + NE], op=ALU.add)],
             reads=[r_psp[q], r_carry, r_w[q]], writes=[r_carry])
        dv(lambda e, L=L: [e.tensor_scalar(out=L(5), in0=L(4), scalar1=float(CAP), scalar2=None, op0=ALU.is_lt)])
        dv(lambda e, L=L: [e.tensor_tensor(out=L(5), in0=L(5), in1=L(1), op=ALU.mult)])
        dv(lambda e, L=L: [e.tensor_tensor(out=L(6), in0=L(4), in1=iotaE[:], op=ALU.add)], reads=[r_iotaE])
        dv(lambda e, L=L: [e.tensor_tensor(out=L(6), in0=L(6), in1=L(5), op=ALU.mult)])
        dv(lambda e, L=L, M=M: [e.max(out=M(8, 16), in_=L(6))])
        for k in range(4):
            dv(lambda e, L=L, M=M, k=k: [e.tensor_scalar(out=L(7), in0=L(6), scalar1=M(8 + k, 9 + k), scalar2=None, op0=ALU.is_equal)])
            dv(lambda e, L=L: [e.tensor_tensor(out=L(7), in0=L(7), in1=L(3), op=ALU.mult)])
            dv(lambda e, L=L, M=M, k=k: [e.tensor_reduce(out=M(20 + k, 21 + k), in_=L(7), axis=mybir.AxisListType.X, op=ALU.add)])
        P.op("dve", lambda e, q=q, M=M: [e.tensor_copy(out=si4[:, q, :], in_=M(8, 12))], reads=[r_w[q]], writes=[r_si[q]])
        P.op("dve", lambda e, q=q, M=M, ti=ti: [e.tensor_copy(out=sc[:, q, :, 1], in_=M(20, 24))], reads=[r_w[q]], writes=[r_sc[q]])
        P.op("dve", lambda e, q=q, ti=ti: [e.tensor_copy(out=sc[:, q, :, 0], in_=tokid[:, ti:ti + 1].broadcast_to([128, 4]))],
             reads=[r_tokid, r_sc[q]], writes=[r_sc[q]])
        for k in range(4):
            P.op("pool", lambda e, q=q, k=k: [e.indirect_dma_start(out=tab_d[:, :], out_offset=bass.IndirectOffsetOnAxis(ap=si4[:, q, k:k + 1], axis=0),
                                                                   in_=sc[:, q, k, :], in_offset=None)],
                 reads=[r_si[q], r_sc[q], r_tab], writes=[r_tab], dma_res=r_sc[q], n_dma=1)
        st.dma(sidx_d[ti], si4[:, q, :], reads=[r_si[q]], res=r_si[q], eng="pool")
        P.op("pe", lambda e, q=q, L=L: [e.transpose(out=ps_g[q][0:NE, 0:128], in_=L(3), identity=idf[:])], reads=[r_w[q], r_idf], writes=[r_psg[q]])
        P.op("act", lambda e, q=q: [e.activation(out=GT[:, q, :], in_=ps_g[q][0:NE, 0:128], func=AF.Copy)], reads=[r_psg[q]], writes=[r_GT[q]])
        st.dma(GT_d[:, ti * 128:(ti + 1) * 128], GT[:, q, :], reads=[r_GT[q]], res=r_GT[q], eng="pool")

    for idx in range(25):
        if idx < 24:
            part(idx, "A")
        if idx >= 1:
            part(idx - 1, "B")
    st.finish()


def stage_experts(nc, tab_d, x2b_d, w_gu, w_down, bgu_d, Y_d, ident_d, n_exp=NE):
    st = Stage(nc, "s5")
    P = st.P
    idf = st.sb("idf", [128, 128], F32)
    idb = st.sb("idb", [128, 128], BF16)
    bgu = st.sb("bgu", [128, NE, 32], F32)
    tb = st.sb("tb", [128, 2, 4, 2], F32)
    ti4 = st.sb("ti4", [128, 2, 4], I32)
    xg = st.sb("xg", [128, 4, D], BF16)
    XT = st.sb("XT", [128, 16, 512], BF16)
    S = st.sb("S", [128, 4, 16 * 256], F32)
    W = st.sb("W", [128, 3, 2, 16, 256], BF16)
    actT = st.sb("actT", [128, 16, 512], BF16)
    Y = st.sb("Y", [128, 4, D], BF16)
    tmp = st.sb("tmp", [128, 2, 4, 512], F32)
    ps_t = [st.ps(BF16, 1024) for _ in range(2)]
    ps_g = [st.ps() for _ in range(2)]
    ps_u = [st.ps() for _ in range(2)]
    ps_y = [st.ps() for _ in range(2)]
    R = lambda n: Res(n)
    r_idf, r_idb, r_bgu, r_XT, r_actT, r_Y = R("idf"), R("idb"), R("bgu"), R("XT"), R("actT"), R("Y")
    r_tb = [R(f"tb{i}") for i in range(2)]
    r_ti = [R(f"ti{i}") for i in range(2)]
    r_xg = [R(f"xg{i}") for i in range(4)]
    r_S = [R(f"S{i}") for i in range(4)]
    r_W = [[R(f"W{i}_{h}") for h in range(2)] for i in range(3)]
    r_tmp = [R(f"tmp{i}") for i in range(2)]
    r_pst = [R(f"pst{i}") for i in range(2)]
    r_psg = [R(f"psg{i}") for i in range(2)]
    r_psu = [R(f"psu{i}") for i in range(2)]
    r_psy = [R(f"psy{i}") for i in range(2)]
    pairs = [(ex, kind, i) for ex in range(n_exp) for (kind, cnt) in (("gu", 8), ("d", 4)) for i in range(cnt)]
    NP = len(pairs)
    CAST_ENG = ("act", "dve", "pool", "act", "dve", "act", "pool", "dve", "act", "pool", "act", "dve")

    def src(pr, h):
        ex, kind, i = pr
        if kind == "gu":
            return w_gu[ex].rearrange("(k p) n -> p k n", p=128)[:, :, h * 2048 + i * 256:h * 2048 + (i + 1) * 256]
        return w_down[ex].rearrange("(k p) n -> p k n", p=128)[:, :, i * 512 + h * 256:i * 512 + (h + 1) * 256]

    def load(pi):
        if _SUB.get("noload") and pi >= 4:
            return
        for h in range(2):
            si = (pi % 2) * 2 + h
            st.dma(S[:, si, :].rearrange("p (k n) -> p k n", k=16), src(pairs[pi], h), writes=[r_S[si]], res=r_S[si], eng="sp")

    def cast(pi):
        for h in range(2):
            si, wi = (pi % 2) * 2 + h, pi % 3
            eng = CAST_ENG[(2 * pi + h) % 12]
            outs = [W[:, wi, h, 8 * i:8 * i + 8, :].rearrange("p k n -> p (k n)") for i in range(2)]
            ins = [S[:, si, 2048 * i:2048 * (i + 1)] for i in range(2)]
            if eng == "act":
                P.op("act", lambda e, outs=outs, ins=ins: [e.activation(out=o, in_=i_, func=AF.Copy) for o, i_ in zip(outs, ins)],
                     reads=[r_S[si]], writes=[r_W[wi][h]])
            else:
                P.op(eng, lambda e, outs=outs, ins=ins: [e.tensor_copy(out=o, in_=i_) for o, i_ in zip(outs, ins)],
                     reads=[r_S[si]], writes=[r_W[wi][h]])

    def prep_tokens(ex):
        q = ex % 2
        s0 = ex * CAP + 1
        st.dma(tb[:, q, :, :], tab_d[s0:s0 + CAP, :].rearrange("(m p) c -> p m c", p=128), writes=[r_tb[q]], res=r_tb[q], eng="act")
        P.op("dve", lambda e, q=q: [e.tensor_copy(out=ti4[:, q, :], in_=tb[:, q, :, 0])], reads=[r_tb[q]], writes=[r_ti[q]])
        for m in range(4):
            P.op("pool", lambda e, q=q, m=m: [e.indirect_dma_start(out=xg[:, m, :], out_offset=None, in_=x2b_d[:, :],
                                                                   in_offset=bass.IndirectOffsetOnAxis(ap=ti4[:, q, m:m + 1], axis=0))],
                 reads=[r_ti[q]], writes=[r_xg[m]], dma_res=r_xg[m], n_dma=1)

    def do_transposes(ex):
        for k2 in range(8):
            b = k2 % 2
            P.op("pe", lambda e, k2=k2, b=b: [e.transpose(out=ps_t[b][:, (kk * 4 + m) * 128:(kk * 4 + m + 1) * 128],
                                                          in_=xg[:, m, (2 * k2 + kk) * 128:(2 * k2 + kk + 1) * 128], identity=idb[:])
                                              for kk in range(2) for m in range(4)], reads=[r_idb] + r_xg, writes=[r_pst[b]])
            st.evac(XT[:, 2 * k2:2 * k2 + 2, :], ps_t[b][:, :].rearrange("p (k n) -> p k n", k=2), reads=[r_pst[b]], writes=[r_XT])

    def compute_gu(ex, j2, wi):
        for jj in range(2):
            j = 2 * j2 + jj
            pb = j % 2
            P.op("pe", lambda e, wi=wi, jj=jj, pb=pb: [e.matmul(ps_g[pb][:, :], lhsT=W[:, wi, 0, k, jj * 128:(jj + 1) * 128], rhs=XT[:, k, :],
                                                                start=(k == 0), stop=(k == 15)) for k in range(16)],
                 reads=[r_W[wi][0], r_XT], writes=[r_psg[pb]])
            P.op("pe", lambda e, wi=wi, jj=jj, pb=pb: [e.matmul(ps_u[pb][:, :], lhsT=W[:, wi, 1, k, jj * 128:(jj + 1) * 128], rhs=XT[:, k, :],
                                                                start=(k == 0), stop=(k == 15)) for k in range(16)],
                 reads=[r_W[wi][1], r_XT], writes=[r_psu[pb]])
            T = lambda i, pb=pb: tmp[:, pb, i, :]
            rt = [r_tmp[pb]]
            P.op("dve", lambda e, T=T, pb=pb, ex=ex, j=j: [e.tensor_scalar(out=T(0), in0=ps_g[pb][:, :], scalar1=bgu[:, ex, j:j + 1], scalar2=7.0,
                                                                            op0=ALU.add, op1=ALU.min)], reads=[r_psg[pb], r_bgu] + rt, writes=rt)
            P.op("act", lambda e, T=T: [e.activation(out=T(1), in_=T(0), func=AF.Sigmoid, scale=1.702)], reads=rt, writes=rt)
            P.op("dve", lambda e, T=T, pb=pb, ex=ex, j=j: [e.tensor_scalar(out=T(2), in0=ps_u[pb][:, :], scalar1=bgu[:, ex, 16 + j:17 + j], scalar2=7.0,
                                                                            op0=ALU.add, op1=ALU.min)], reads=[r_psu[pb], r_bgu] + rt, writes=rt)
            P.op("dve", lambda e, T=T: [e.tensor_scalar(out=T(2), in0=T(2), scalar1=-7.0, scalar2=1.0, op0=ALU.max, op1=ALU.add)], reads=rt, writes=rt)
            P.op("pool", lambda e, T=T: [e.tensor_tensor(out=T(3), in0=T(0), in1=T(1), op=ALU.mult)], reads=rt, writes=rt)
            P.op("pool", lambda e, T=T, j=j: [e.tensor_tensor(out=actT[:, j, :], in0=T(3), in1=T(2), op=ALU.mult)], reads=rt, writes=rt + [r_actT])

    def compute_d(ex, n, wi):
        q = ex % 2
        for m in range(4):
            pb = m % 2
            P.op("pe", lambda e, wi=wi, m=m, pb=pb: [e.matmul(ps_y[pb][:, h * 256:(h + 1) * 256], lhsT=actT[:, k, m * 128:(m + 1) * 128],
                                                              rhs=W[:, wi, h, k, :], start=(k == 0), stop=(k == 15))
                                                     for h in range(2) for k in range(16)],
                 reads=[r_W[wi][0], r_W[wi][1], r_actT], writes=[r_psy[pb]])
            P.op("act", lambda e, m=m, n=n, pb=pb, q=q: [e.activation(out=Y[:, m, n * 512:(n + 1) * 512], in_=ps_y[pb][:, :], func=AF.Copy,
                                                                      scale=tb[:, q, m, 1:2])], reads=[r_psy[pb], r_tb[q]], writes=[r_Y])

    load(0)
    load(1)
    st.dma(idf[:], ident_d, writes=[r_idf], res=r_idf, eng="act")
    st.dma(bgu[:, :, :], bgu_d, writes=[r_bgu], res=r_bgu, eng="act")
    P.op("dve", lambda e: [e.tensor_copy(out=idb[:], in_=idf[:])], reads=[r_idf], writes=[r_idb])
    prep_tokens(0)
    cast(0)
    load(2)
    cast(1)
    load(3)
    do_transposes(0)
    for pi, (ex, kind, i) in enumerate(pairs):
        if pi + 2 < NP:
            cast(pi + 2)
        if pi + 4 < NP:
            load(pi + 4)
        if kind == "gu":
            compute_gu(ex, i, pi % 3)
            if i == 1 and ex + 1 < n_exp:
                prep_tokens(ex + 1)
            if i == 7 and ex + 1 < n_exp:
                do_transposes(ex + 1)
        else:
            compute_d(ex, i, pi % 3)
            if i == 3:
                s0 = ex * CAP + 1
                st.dma(Y_d[s0:s0 + CAP, :].rearrange("(m p) d -> p m d", p=128), Y[:, :, :], reads=[r_Y], res=r_Y, eng="act")
    st.finish()


def stage_combine(nc, x2_d, Y_d, sidx_d, GT_d, bdown_d, gam_d, bet_d, out_d):
    st = Stage(nc, "s6")
    P = st.P
    NB = 3
    gam = st.sb("gam", [128, D], F32)
    bet = st.sb("bet", [128, D], F32)
    bd = st.sb("bd", [32, D], F32)
    si4 = st.sb("si4", [128, NB, 4], I32)
    GT = st.sb("GT", [32, NB, 128], F32)
    xr = st.sb("xr", [128, NB, D], F32)
    y = st.sb("y", [128, NB, D], F32)
    Yg = st.sb("Yg", [128, NB, 4, D], BF16)
    stats = st.sb("stats", [128, NB, 4, 6], F32)
    mv = st.sb("mv", [128, NB, 4], F32)
    ps = [st.ps() for _ in range(8)]
    R = lambda n: Res(n)
    r_g, r_b, r_bd = R("g"), R("b"), R("bd")
    r_si = [R(f"si{i}") for i in range(NB)]
    r_GT = [R(f"GT{i}") for i in range(NB)]
    r_xr = [R(f"xr{i}") for i in range(NB)]
    r_y = [R(f"y{i}") for i in range(NB)]
    r_Yg = [[R(f"Yg{i}_{k}") for k in range(4)] for i in range(NB)]
    r_st = [R(f"st{i}") for i in range(NB)]
    r_ps = [R(f"ps{i}") for i in range(8)]
    st.dma(gam[:], gam_d, writes=[r_g], res=r_g)
    st.dma(bet[:], bet_d, writes=[r_b], res=r_b)
    st.dma(bd[:, :], bdown_d, writes=[r_bd], res=r_bd)
    for ti in range(24):
        q = ti % NB
        pq = ti % 2
        r0 = ti * 128
        st.dma(si4[:, q, :], sidx_d[ti], writes=[r_si[q]], res=r_si[q])
        st.dma(GT[:, q, :], GT_d[:, r0:r0 + 128], writes=[r_GT[q]], res=r_GT[q])
        st.dma(xr[:, q, :], x2_d[r0:r0 + 128, :], writes=[r_xr[q]], res=r_xr[q])
        for k in range(4):
            P.op("pool", lambda e, q=q, k=k: [e.indirect_dma_start(out=Yg[:, q, k, :], out_offset=None, in_=Y_d[:, :],
                                                                   in_offset=bass.IndirectOffsetOnAxis(ap=si4[:, q, k:k + 1], axis=0))],
                 reads=[r_si[q]], writes=[r_Yg[q][k]], dma_res=r_Yg[q][k], n_dma=1)
        for n in range(4):
            b = pq * 4 + n
            P.op("pe", lambda e, q=q, n=n, b=b: [e.matmul(ps[b][:, :], lhsT=GT[:, q, :], rhs=bd[:, n * 512:(n + 1) * 512], start=True, stop=True)],
                 reads=[r_GT[q], r_bd], writes=[r_ps[b]])
            P.op("dve", lambda e, n=n, b=b, q=q: [e.scalar_tensor_tensor(out=y[:, q, n * 512:(n + 1) * 512], in0=xr[:, q, n * 512:(n + 1) * 512],
                                                                         scalar=ALPHA, in1=ps[b][:, :], op0=ALU.mult, op1=ALU.add)],
                 reads=[r_xr[q], r_ps[b]], writes=[r_y[q]])
        for k in range(4):
            P.op("dve", lambda e, q=q, k=k: [e.tensor_tensor(out=y[:, q, :], in0=y[:, q, :], in1=Yg[:, q, k, :], op=ALU.add)],
                 reads=[r_y[q], r_Yg[q][k]], writes=[r_y[q]])
        ln_tail(st, y[:, q, :], stats[:, q, :, :], mv[:, q, :], gam, bet, r_y[q], r_st[q], [r_g, r_b])
        st.dma(out_d[r0:r0 + 128, :], y[:, q, :], reads=[r_y[q]], res=r_y[q], eng="pool")
    st.finish()


def build_program(upto=99, n_exp=NE, dbg=(), only=None, feed=()):
    nc = bass.Bass("TRN2", target_bir_lowering=False)
    dt = nc.dram_tensor

    def inp(name, shape, dtype=F32):
        return dt(name, shape, dtype, kind="ExternalInput").ap()

    def scr(name, shape, dtype):
        kind = "ExternalInput" if name in feed else ("ExternalOutput" if name in dbg else "Internal")
        return dt(name, shape, dtype, kind=kind).ap()

    def run(k):
        return (k in only) if only is not None else (upto >= k)

    ident = inp("ident", [128, 128])
    x1_d = scr("x1_d", [NOWN, D], F32)
    if run(1) or run(2):
        xs = inp("xs", [NSLAB, D])
        w_in = inp("w_in", [D, 4096])
        AT_d = scr("AT_d", [8, 128, NSLAB], BF16)
        QT_d = scr("QT_d", [8, 128, NSLAB], BF16)
        KT_d = scr("KT_d", [8, 128, NSLAB], BF16)
        V_d = scr("V_d", [NSLAB, 1024], BF16)
    if run(1):
        stage_proj(nc, "s1a", xs, NSLAB, w_in, 0, 2048, [(0, 8, AT_d, None), (1024, 8, QT_d, 0.125)], [], ident)
        stage_proj(nc, "s1b", xs, NSLAB, w_in, 2048, 2048, [(0, 8, KT_d, None)], [(1024, 1024, V_d)], ident)
    if run(2):
        w_pool = inp("w_pool", [4, 256, 256])
        pscale = inp("pscale", [128, 8])
        g2 = inp("g2", [128, 16, 14 * 64])
        rm = inp("rm", [2, 288])
        e2 = inp("e2", [2, 128])
        invc = inp("invc", [4, NOWN])
        w_out = inp("w_out", [D, D])
        ln1g, ln1b = inp("ln1g", [128, D]), inp("ln1b", [128, D])
        xown = inp("xown", [NOWN, D])
        mix_d = scr("mix_d", [16, 128, NOWN], BF16)
        stage_mix(nc, AT_d, QT_d, KT_d, V_d, mix_d, w_pool, pscale, g2, rm, e2, invc, ident)
        stage_proj_ln(nc, "s2b", mix_d, w_out, xown, ln1g, ln1b, x1_d)
    x2_d = scr("x2_d", [NOWN, D], F32)
    x2b_d = scr("x2b_d", [NOWN + 1, D], BF16)
    if run(3):
        memc = inp("memc", [512, D])
        w_xq, w_xkv, w_xo = inp("w_xq", [D, D]), inp("w_xkv", [D, 4096]), inp("w_xo", [D, D])
        ln2g, ln2b = inp("ln2g", [128, D]), inp("ln2b", [128, D])
        KTm_d = scr("KTm_d", [16, 128, 512], BF16)
        Vm_d = scr("Vm_d", [512, D], BF16)
        QX_d = scr("QX_d", [16, 128, NOWN], BF16)
        oT_d = scr("oT_d", [16, 128, NOWN], BF16)
        sub = only_sub if (only_sub := _SUB.get("s3")) else ("k", "v", "q", "x", "d")
        if "k" in sub:
            stage_proj(nc, "s3k", memc, 512, w_xkv, 0, 2048, [(0, 16, KTm_d, None)], [], ident)
        if "v" in sub:
            stage_proj(nc, "s3v", memc, 512, w_xkv, 2048, 2048, [], [(0, 2048, Vm_d)], ident)
        if "q" in sub:
            stage_proj(nc, "s3q", x1_d, NOWN, w_xq, 0, 2048, [(0, 16, QX_d, 512.0 ** -0.5)], [], ident)
        if "x" in sub:
            stage_xattn(nc, QX_d, KTm_d, Vm_d, oT_d)
        if "d" in sub:
            stage_proj_ln(nc, "s3d", oT_d, w_xo, x1_d, ln2g, ln2b, x2_d, out16=x2b_d)
    tab_d = scr("tab_d", [NSLOT + 1, 2], F32)
    sidx_d = scr("sidx_d", [24, 128, 4], I32)
    GT_d = scr("GT_d", [NE, NOWN], F32)
    Y_d = scr("Y_d", [NSLOT + 1, D], BF16)
    if run(4):
        w_router = inp("w_router", [D, NE])
        brep = inp("brep", [128, NE])
        triu = inp("triu", [128, 128])
        iotaE = inp("iotaE", [128, NE])
        tokid = inp("tokid", [128, 24])
        tabinit = inp("tabinit", [128, NSLOT // 128 + 1, 2])
        stage_router(nc, x2_d, w_router, brep, ident, triu, iotaE, tokid, tab_d, tabinit, sidx_d, GT_d, None, Y_d, x2b_d)
    if run(5):
        w_gu = inp("w_gu", [n_exp, D, 4096])
        w_down = inp("w_down", [n_exp, D, D])
        bgu = inp("bgu", [128, NE, 32])
        stage_experts(nc, tab_d, x2b_d, w_gu, w_down, bgu, Y_d, ident, n_exp=n_exp)
    if run(6):
        bdown = inp("bdown", [NE, D])
        ln3g, ln3b = inp("ln3g", [128, D]), inp("ln3b", [128, D])
        out_d = dt("out", [NOWN, D], F32, kind="ExternalOutput").ap()
        stage_combine(nc, x2_d, Y_d, sidx_d, GT_d, bdown, ln3g, ln3b, out_d)
    return nc


_SUB = {}


def _rep(v):
    return np.ascontiguousarray(np.broadcast_to(np.asarray(v, np.float32)[None, :], (128, v.shape[0])))


def make_inputs(inp, upto=99, n_exp=NE):
    f = lambda k: np.asarray(inp[k], np.float32)[0]
    x_p, x_s = np.asarray(inp["x_prompt"], np.float32), np.asarray(inp["x_sample"], np.float32)
    mem_p, mem_s = np.asarray(inp["mem_prompt"], np.float32), np.asarray(inp["mem_sample"], np.float32)
    rpb = f("rpb")
    kc = np.arange(64)
    qc = np.arange(64)
    cstart = np.clip(qc - 8, 0, 48)
    cvalid = (kc[:, None] >= cstart[None, :]) & (kc[:, None] < cstart[None, :] + 16)
    dcidx = np.clip(kc[:, None] - qc[None, :] + 15, 0, 30)
    g2 = np.empty((2, 64, 16, 14, 64), np.float32)
    for e in range(2):
        for Di in range(14):
            dr = (10 - Di) + 3 + e
            vals = rpb[:, dr, :][:, dcidx]
            g2[e, :, :, Di, :] = np.where(cvalid[None], vals, np.float32(-1e30)).transpose(1, 0, 2)
    g2 = np.ascontiguousarray(g2.reshape(128, 16, 14 * 64))
    e2 = np.zeros((2, 128), np.float32)
    e2[0, :64] = 1
    e2[1, 64:] = 1
    ident = np.eye(128, dtype=np.float32)
    triu = np.triu(np.ones((128, 128), np.float32), 1)
    iotaE = _rep(np.arange(NE, dtype=np.float32) * CAP + 1)
    tokid = np.ascontiguousarray((np.arange(24)[None, :] * 128 + np.arange(128)[:, None]).astype(np.float32))
    tabinit = np.zeros((128, NSLOT // 128 + 1, 2), np.float32)
    tabinit[:, :, 0] = NOWN
    shared = dict(ident=ident, w_in=f("w_in"), w_pool=f("w_pool"), g2=g2, e2=e2, w_out=f("w_out"),
                  pscale=np.ascontiguousarray(f("pool_scale").reshape(8, 128).T), ln1g=_rep(f("ln1_g")), ln1b=_rep(f("ln1_b")))
    if upto >= 3:
        shared.update(w_xq=f("w_xq"), w_xkv=f("w_xkv"), w_xo=f("w_xo"), ln2g=_rep(f("ln2_g")), ln2b=_rep(f("ln2_b")))
    if upto >= 4:
        shared.update(w_router=f("w_router"), brep=_rep(f("b_router")), triu=triu, iotaE=iotaE, tokid=tokid, tabinit=tabinit)
    if upto >= 5:
        bgu = np.ascontiguousarray(f("b_gu").reshape(NE, 32, 128).transpose(2, 0, 1))
        shared.update(w_gu=f("w_gu")[:n_exp], w_down=f("w_down")[:n_exp], bgu=bgu)
    if upto >= 6:
        shared.update(bdown=f("b_down"), ln3g=_rep(f("ln3_g")), ln3b=_rep(f("ln3_b")))
    maps = []
    for c in range(8):
        pb, ph = c // 2, c % 2
        xs = np.zeros((64, 64, D), np.float32)
        rm = np.zeros((2, 12, 6, 4), np.float32)
        invc = np.zeros((4, 48, 64), np.float32)
        own = []
        for (src, R, cs, nrow, srow0, blk0) in ((x_p[pb].reshape(64, 64, D), 64, 32 * ph, 32, 0, 0),
                                                (x_s[0].reshape(128, 64, D), 128, 16 * c, 16, 40, 8)):
            lo, hi = cs - 4, cs + nrow + 4
            a, b = max(lo, 0), min(hi, R)
            xs[srow0 + (a - lo):srow0 + (b - lo)] = src[a:b]
            own.append(src[cs:cs + nrow].reshape(nrow * 64, D))
            for bb in range(nrow // 4):
                for j in range(12):
                    for t in range(4):
                        r = cs + 4 * bb + t
                        kr = cs + 4 * bb - 4 + j
                        rs = min(max(r - 4, 0), R - 8)
                        ok = (0 <= kr < R) and (rs <= kr <= rs + 7)
                        rm[j % 2, blk0 + bb, j // 2, t] = 0.0 if ok else -1e30
            L = R * 64
            tpos = np.arange(cs * 64, (cs + nrow) * 64)
            orow0 = 0 if blk0 == 0 else 32
            for g, w in enumerate((2, 4, 8, 16)):
                lo_ = np.clip(tpos - w // 2, 0, L)
                hi_ = np.clip(tpos - w // 2 + w, 0, L)
                invc[g, orow0:orow0 + nrow] = (np.float32(1.0) / (hi_ - lo_).astype(np.float32)).reshape(nrow, 64)
        m = dict(shared)
        m.update(xs=xs.reshape(NSLAB, D), rm=np.ascontiguousarray(rm.reshape(2, 288)), invc=np.ascontiguousarray(invc.reshape(4, NOWN)),
                 xown=np.ascontiguousarray(np.concatenate(own, 0)))
        if upto >= 3:
            m["memc"] = np.ascontiguousarray(np.concatenate([mem_p[pb], mem_s[0]], 0))
        maps.append(m)
    return maps


_NC = {}


def kernel(**inputs):
    if "nc" not in _NC:
        _NC["nc"] = build_program()
    nc = _NC["nc"]
    maps = make_inputs(inputs)
    res = run_bass_kernel_spmd(nc, maps, core_ids=list(range(8)))
    y_p = np.empty((4, 4096, D), np.float32)
    y_s = np.empty((1, 8192, D), np.float32)
    for c in range(8):
        o = np.asarray(res.results[c]["out"])
        pb, ph = c // 2, c % 2
        y_p[pb, ph * 2048:(ph + 1) * 2048] = o[:2048]
        y_s[0, c * 1024:(c + 1) * 1024] = o[2048:]
    return (y_p, y_s)
```

```python
from contextlib import ExitStack
import numpy as np
import concourse.bass as bass
import concourse.mybir as mybir
from concourse.bass_utils import run_bass_kernel_spmd

F32 = mybir.dt.float32
BF16 = mybir.dt.bfloat16
I32 = mybir.dt.int32
AF = mybir.ActivationFunctionType
ALU = mybir.AluOpType

D = 2048
NSLAB = 4096
NOWN = 3072
NBLK = 6
NE = 32
CAP = 512
NSLOT = NE * CAP
ALPHA = 2.0 ** 0.25
EPS = 1e-5
ENGS = ("pe", "act", "dve", "pool", "sp")


class Res:
    __slots__ = ("name", "last_w", "readers", "sem", "cnt")

    def __init__(self, name):
        self.name = name
        self.last_w = None
        self.readers = []
        self.sem = None
        self.cnt = 0


class Op:
    __slots__ = ("eng", "emit", "deps", "dma_res", "n_dma", "tok_sem", "tok_val", "needed")


class Prog:
    def __init__(self, nc, tag):
        self.nc = nc
        self.tag = tag
        self.ops = []
        self.by_eng = {e: [] for e in ENGS}

    def op(self, eng, emit, reads=(), writes=(), dma_res=None, n_dma=0):
        o = Op()
        o.eng, o.emit, o.dma_res, o.n_dma, o.needed = eng, emit, dma_res, n_dma, False
        o.tok_val = None
        deps = set()
        for r in reads:
            if r.last_w is not None:
                deps.add(r.last_w)
        for w in writes:
            if w.last_w is not None:
                deps.add(w.last_w)
            deps.update(w.readers)
        for r in reads:
            r.readers.append(o)
        for w in writes:
            w.last_w = o
            w.readers = []
        deps.discard(o)
        if eng == "pe" and dma_res is None:
            deps = {d for d in deps if not (d.eng == "pe" and d.dma_res is None)}
        o.deps = deps
        for d in deps:
            d.needed = True
        if dma_res is not None:
            dma_res.cnt += 16 * n_dma
            o.tok_val = dma_res.cnt
        self.ops.append(o)
        self.by_eng[eng].append(o)
        return o

    def barrier(self):
        last = []
        for e in ENGS:
            for o in reversed(self.by_eng[e]):
                if o.dma_res is None and o.emit is not None:
                    last.append(o)
                    break
        dmas = {}
        for o in self.ops:
            if o.dma_res is not None:
                dmas[id(o.dma_res)] = o
        alld = last + list(dmas.values())
        for d in alld:
            d.needed = True
        for e in ENGS:
            o = Op()
            o.eng, o.emit, o.dma_res, o.n_dma, o.needed = e, None, None, 0, False
            o.tok_val = None
            o.deps = set(alld)
            self.ops.append(o)
            self.by_eng[e].append(o)

    def assign(self, sems):
        self.eng_sem = {e: sems(f"{self.tag}_e_{e}") for e in ENGS}
        ms = {e: 0 for e in ENGS}
        for o in self.ops:
            if o.dma_res is not None:
                if o.dma_res.sem is None:
                    o.dma_res.sem = sems(f"{self.tag}_d_{o.dma_res.name}")
                o.tok_sem = o.dma_res.sem
            else:
                o.tok_sem = self.eng_sem[o.eng]
                if o.needed:
                    ms[o.eng] += 1
                    o.tok_val = ms[o.eng]

    def emit(self, block):
        def run_engine(e, eng):
            known = {}
            for o in self.by_eng[e]:
                waits = {}
                for d in o.deps:
                    k = id(d.tok_sem)
                    v = d.tok_val
                    if known.get(k, 0) >= v:
                        continue
                    if k not in waits or waits[k][1] < v:
                        waits[k] = (d.tok_sem, v)
                for k, (s, v) in waits.items():
                    eng.wait_ge(s, v)
                    known[k] = v
                if o.emit is None:
                    continue
                instrs = o.emit(eng)
                if o.dma_res is not None:
                    assert len(instrs) == o.n_dma, (len(instrs), o.n_dma)
                    for ins in instrs:
                        ins.then_inc(o.tok_sem, 16)
                elif o.needed:
                    instrs[-1].then_inc(o.tok_sem, 1)

        @block.tensor
        def _(eng):
            run_engine("pe", eng)

        @block.scalar
        def _(eng):
            run_engine("act", eng)

        @block.vector
        def _(eng):
            run_engine("dve", eng)

        @block.gpsimd
        def _(eng):
            run_engine("pool", eng)

        @block.sync
        def _(eng):
            run_engine("sp", eng)


class Stage:
    def __init__(self, nc, tag):
        self.nc, self.tag = nc, tag
        self.stack = ExitStack()
        self.P = Prog(nc, tag)
        self.nps = 0
        self.rr = 0

    def sb(self, name, shape, dt):
        return self.stack.enter_context(self.nc.sbuf_tensor(f"{self.tag}_{name}", shape, dt))

    def ps(self, dt=F32, cols=512):
        self.nps += 1
        assert self.nps <= 8
        return self.stack.enter_context(self.nc.psum_tensor(f"{self.tag}_ps{self.nps}", [128, cols], dt))

    def finish(self):
        self.P.barrier()
        hs = []

        def mk(n):
            hs.append(self.nc.alloc_semaphore(n))
            return hs[-1]
        self.P.assign(mk)
        if _SUB.get("scopes"):
            with self.nc.named_scope(self.tag):
                with self.nc.Block() as block:
                    self.P.emit(block)
        else:
            with self.nc.Block() as block:
                self.P.emit(block)
        self.nc.clear_and_free_semaphores(hs)
        self.nc.all_engine_barrier()
        self.stack.close()

    def dma(self, out, in_, reads=(), writes=(), res=None, eng="sp"):
        self.P.op(eng, lambda e: [e.dma_start(out=out, in_=in_)], reads=reads, writes=writes, dma_res=res, n_dma=1)

    def evac(self, out, in_, reads, writes, scale=None):
        self.rr += 1
        if self.rr % 2 == 0:
            if scale is None:
                self.P.op("act", lambda e: [e.activation(out=out, in_=in_, func=AF.Copy)], reads=reads, writes=writes)
            else:
                self.P.op("act", lambda e: [e.activation(out=out, in_=in_, func=AF.Copy, scale=scale)], reads=reads, writes=writes)
        else:
            if scale is None:
                self.P.op("dve", lambda e: [e.tensor_copy(out=out, in_=in_)], reads=reads, writes=writes)
            else:
                self.P.op("dve", lambda e: [e.tensor_scalar(out=out, in0=in_, scalar1=float(scale), scalar2=None, op0=ALU.mult)],
                          reads=reads, writes=writes)


def load_w_bf16(st, wsb, w_view, r_w, nk_split=4):
    step = 16 // nk_split
    st.P.op("pool", lambda e: [e.dma_start(out=wsb[:, i * step:(i + 1) * step, :], in_=w_view[:, i * step:(i + 1) * step, :])
                               for i in range(nk_split)], writes=[r_w], dma_res=r_w, n_dma=nk_split)


def stage_proj(nc, tag, src, ntok, w, c0, wcols, fm_outs, tm_outs, ident_d):
    st = Stage(nc, tag)
    P = st.P
    wsb = st.sb("w", [128, 16, wcols], BF16)
    ident = st.sb("ident", [128, 128], F32)
    xt = st.sb("xt", [128, 4, D], F32)
    xT = st.sb("xT", [128, 2, 16, 512], BF16)
    ost = st.sb("ost", [128, 4, 512], BF16)
    vst = st.sb("vst", [128, 2, 512], BF16)
    pst = [st.ps() for _ in range(2)]
    psm = [st.ps() for _ in range(6)]
    r_w, r_id = Res("w"), Res("id")
    r_xt = [Res(f"xt{t}") for t in range(4)]
    r_xT = [[Res(f"xT{p}_{k}") for k in range(16)] for p in range(2)]
    r_ost = [Res(f"ost{i}") for i in range(4)]
    r_vst = [Res(f"vst{i}") for i in range(2)]
    r_pst = [Res(f"pst{i}") for i in range(2)]
    r_psm = [Res(f"psm{i}") for i in range(6)]
    st.dma(ident[:], ident_d, writes=[r_id], res=r_id)
    load_w_bf16(st, wsb, w.rearrange("(k p) n -> p k n", p=128)[:, :, c0:c0 + wcols], r_w)
    nblk = ntok // 512
    mi = 0
    oi = 0
    vi = 0
    def xt_loads(blk):
        for t in range(4):
            r0 = blk * 512 + t * 128
            st.dma(xt[:, t, :], src[r0:r0 + 128, :], writes=[r_xt[t]], res=r_xt[t])
    xt_loads(0)
    for blk in range(nblk):
        par = blk % 2
        for k in range(16):
            b = k % 2
            P.op("pe", lambda e, k=k, b=b: [e.transpose(out=pst[b][:, t * 128:(t + 1) * 128], in_=xt[:, t, k * 128:(k + 1) * 128],
                                                       identity=ident[:]) for t in range(4)],
                 reads=[r_id] + r_xt, writes=[r_pst[b]])
            st.evac(xT[:, par, k, :], pst[b][:, :], reads=[r_pst[b]], writes=[r_xT[par][k]])
        if blk + 1 < nblk:
            xt_loads(blk + 1)
        for (coff, nch, dst, scale) in fm_outs:
            for oc in range(nch):
                b = mi % 6
                mi += 1
                P.op("pe", lambda e, oc=oc, b=b, coff=coff, par=par: [
                    e.matmul(psm[b][:, :], lhsT=wsb[:, k, coff + oc * 128:coff + (oc + 1) * 128], rhs=xT[:, par, k, :],
                             start=(k == 0), stop=(k == 15)) for k in range(16)],
                     reads=[r_w] + r_xT[par], writes=[r_psm[b]])
                o = oi % 4
                oi += 1
                st.evac(ost[:, o, :], psm[b][:, :], reads=[r_psm[b]], writes=[r_ost[o]], scale=scale)
                st.dma(dst[oc, :, blk * 512:(blk + 1) * 512], ost[:, o, :], reads=[r_ost[o]], res=r_ost[o])
        for (coff, ncols, dst) in tm_outs:
            for t in range(4):
                for n in range(ncols // 512):
                    b = mi % 6
                    mi += 1
                    P.op("pe", lambda e, t=t, n=n, b=b, coff=coff, par=par: [
                        e.matmul(psm[b][:, :], lhsT=xT[:, par, k, t * 128:(t + 1) * 128],
                                 rhs=wsb[:, k, coff + n * 512:coff + (n + 1) * 512], start=(k == 0), stop=(k == 15))
                        for k in range(16)], reads=[r_w] + r_xT[par], writes=[r_psm[b]])
                    o = vi % 2
                    vi += 1
                    st.evac(vst[:, o, :], psm[b][:, :], reads=[r_psm[b]], writes=[r_vst[o]])
                    r0 = blk * 512 + t * 128
                    st.dma(dst[r0:r0 + 128, n * 512:(n + 1) * 512], vst[:, o, :], reads=[r_vst[o]], res=r_vst[o])
    st.finish()


def stage_proj_ln(nc, tag, actT_d, w, resid, gam_d, bet_d, out32, out16=None):
    st = Stage(nc, tag)
    P = st.P
    NB = 3
    wsb = st.sb("w", [128, 16, D], BF16)
    gam = st.sb("gam", [128, D], F32)
    bet = st.sb("bet", [128, D], F32)
    aT = st.sb("aT", [128, 2, 16, 512], BF16)
    xr = st.sb("xr", [128, NB, D], F32)
    y = st.sb("y", [128, NB, D], F32)
    y16 = st.sb("y16", [128, 2, D], BF16)
    stats = st.sb("stats", [128, NB, 4, 6], F32)
    mv = st.sb("mv", [128, NB, 4], F32)
    ps = [st.ps() for _ in range(8)]
    r_w, r_g, r_b = Res("w"), Res("g"), Res("b")
    r_aT = [Res(f"aT{i}") for i in range(2)]
    r_xr = [Res(f"xr{i}") for i in range(NB)]
    r_y = [Res(f"y{i}") for i in range(NB)]
    r_y16 = [Res(f"y16{i}") for i in range(2)]
    r_st = [Res(f"st{i}") for i in range(NB)]
    r_ps = [Res(f"ps{i}") for i in range(8)]
    load_w_bf16(st, wsb, w.rearrange("(k p) n -> p k n", p=128), r_w)
    st.dma(gam[:], gam_d, writes=[r_g], res=r_g)
    st.dma(bet[:], bet_d, writes=[r_b], res=r_b)
    ti = 0
    for blk in range(NBLK):
        par = blk % 2
        st.dma(aT[:, par, :, :], actT_d.rearrange("c p n -> p c n")[:, :, blk * 512:(blk + 1) * 512], writes=[r_aT[par]], res=r_aT[par])
        for t in range(4):
            q = ti % NB
            pq = ti % 2
            h16 = ti % 2
            ti += 1
            r0 = blk * 512 + t * 128
            st.dma(xr[:, q, :], resid[r0:r0 + 128, :], writes=[r_xr[q]], res=r_xr[q])
            for n in range(4):
                b = pq * 4 + n
                P.op("pe", lambda e, t=t, n=n, b=b, par=par: [
                    e.matmul(ps[b][:, :], lhsT=aT[:, par, k, t * 128:(t + 1) * 128], rhs=wsb[:, k, n * 512:(n + 1) * 512],
                             start=(k == 0), stop=(k == 15)) for k in range(16)], reads=[r_w, r_aT[par]], writes=[r_ps[b]])
                P.op("dve", lambda e, n=n, b=b, q=q: [
                    e.scalar_tensor_tensor(out=y[:, q, n * 512:(n + 1) * 512], in0=xr[:, q, n * 512:(n + 1) * 512], scalar=ALPHA,
                                           in1=ps[b][:, :], op0=ALU.mult, op1=ALU.add)],
                     reads=[r_xr[q], r_ps[b]], writes=[r_y[q]])
            ln_tail(st, y[:, q, :], stats[:, q, :, :], mv[:, q, :], gam, bet, r_y[q], r_st[q], [r_g, r_b])
            st.dma(out32[r0:r0 + 128, :], y[:, q, :], reads=[r_y[q]], res=r_y[q], eng="pool")
            if out16 is not None:
                P.op("act", lambda e, q=q, h16=h16: [e.activation(out=y16[:, h16, :], in_=y[:, q, :], func=AF.Copy)], reads=[r_y[q]], writes=[r_y16[h16]])
                st.dma(out16[r0:r0 + 128, :], y16[:, h16, :], reads=[r_y16[h16]], res=r_y16[h16], eng="pool")
    st.finish()


def ln_tail(st, yv, statv, mvv, gam, bet, r_y, r_st, r_gb):
    P = st.P
    for n in range(4):
        P.op("dve", lambda e, n=n: [e.bn_stats(out=statv[:, n, :], in_=yv[:, n * 512:(n + 1) * 512])], reads=[r_y], writes=[r_st])
    P.op("dve", lambda e: [e.bn_aggr(out=mvv[:, 0:2], in_=statv)], reads=[r_st], writes=[r_st])
    P.op("act", lambda e: [e.activation(out=mvv[:, 2:3], in_=mvv[:, 1:2], func=AF.Sqrt, bias=EPS, scale=1.0)], reads=[r_st], writes=[r_st])
    P.op("dve", lambda e: [e.reciprocal(out=mvv[:, 2:3], in_=mvv[:, 2:3])], reads=[r_st], writes=[r_st])
    P.op("dve", lambda e: [e.scalar_tensor_tensor(out=mvv[:, 3:4], in0=mvv[:, 0:1], scalar=-1.0, in1=mvv[:, 2:3],
                                                  op0=ALU.mult, op1=ALU.mult)], reads=[r_st], writes=[r_st])
    P.op("act", lambda e: [e.activation(out=yv, in_=yv, func=AF.Identity, bias=mvv[:, 3:4], scale=mvv[:, 2:3])],
         reads=[r_st, r_y], writes=[r_y])
    P.op("dve", lambda e: [e.tensor_tensor(out=yv, in0=yv, in1=gam[:], op=ALU.mult)], reads=[r_y, r_gb[0]], writes=[r_y])
    P.op("pool", lambda e: [e.tensor_tensor(out=yv, in0=yv, in1=bet[:], op=ALU.add)], reads=[r_y, r_gb[1]], writes=[r_y])


def ks_of(b):
    return 4 * b if b < 8 else 4 * b + 8


def stage_mix(nc, AT_d, QT_d, KT_d, V_d, mix_d, w_pool, pscale_d, g2_d, rm_d, e2_d, invc_d, ident_d):
    st = Stage(nc, "s2a")
    P = st.P
    wp = st.sb("wp", [128, 4, 2, 256], BF16)
    psc = st.sb("psc", [128, 8], F32)
    g2f = st.sb("g2f", [128, 2, 14 * 64], F32)
    g2 = st.sb("g2", [128, 16, 14 * 64], BF16)
    idf = st.sb("idf", [128, 128], F32)
    idb = st.sb("idb", [128, 128], BF16)
    e2f = st.sb("e2f", [2, 128], F32)
    e2 = st.sb("e2", [2, 128], BF16)
    rmf = st.sb("rmf", [2, 288], F32)
    rm = st.sb("rm", [2, 288], BF16)
    ones = st.sb("ones", [128, 64], BF16)
    aT = st.sb("aT", [128, 8, 544], BF16)
    invc = st.sb("invc", [128, 4, 512], F32)
    T = st.sb("T", [128, 4, 544], F32)
    pT = st.sb("pT", [128, 8, 512], BF16)
    mixT = st.sb("mixT", [128, 2, 16, 512], BF16)
    KT = st.sb("KT", [128, 2, 1024], BF16)
    QT = st.sb("QT", [128, 2, 512], BF16)
    V = st.sb("V", [128, 2, 8, 128], BF16)
    PT = st.sb("PT", [128, 4, 512], BF16)
    rec = st.sb("rec", [128, 2, 256], F32)
    ps_s = [st.ps() for _ in range(3)]
    ps_o = [st.ps() for _ in range(2)]
    ps_d = [st.ps() for _ in range(2)]
    ps_p = st.ps()
    R = lambda n: Res(n)
    r_wp, r_psc, r_g2f, r_g2, r_idf, r_idb, r_e2f, r_e2, r_rmf, r_rm, r_ones = (R(n) for n in
        ("wp", "psc", "g2f", "g2", "idf", "idb", "e2f", "e2", "rmf", "rm", "ones"))
    r_aT, r_invc, r_T = R("aT"), R("invc"), R("T")
    r_mix = [R("mixT0"), R("mixT1")]
    r_pT = [R(f"pT{c}") for c in range(8)]
    r_hp = [R(f"hp{i}") for i in range(2)]
    r_PT = [R(f"PT{i}") for i in range(4)]
    r_rec = [R(f"rec{i}") for i in range(2)]
    r_pss = [R(f"pss{i}") for i in range(3)]
    r_pso = [R(f"pso{i}") for i in range(2)]
    r_psd = [R(f"psd{i}") for i in range(2)]
    r_psp = R("psp")
    P.op("pool", lambda e: [e.dma_start(out=wp[:], in_=w_pool.rearrange("g (cc p) d -> p g cc d", p=128))], writes=[r_wp], dma_res=r_wp, n_dma=1)
    st.dma(psc[:], pscale_d, writes=[r_psc], res=r_psc)
    st.dma(idf[:], ident_d, writes=[r_idf], res=r_idf)
    st.dma(e2f[:], e2_d, writes=[r_e2f], res=r_e2f)
    st.dma(rmf[:], rm_d, writes=[r_rmf], res=r_rmf)
    P.op("dve", lambda e: [e.tensor_copy(out=idb[:], in_=idf[:])], reads=[r_idf], writes=[r_idb])
    P.op("dve", lambda e: [e.tensor_copy(out=e2[:], in_=e2f[:])], reads=[r_e2f], writes=[r_e2])
    P.op("dve", lambda e: [e.tensor_copy(out=rm[:], in_=rmf[:])], reads=[r_rmf], writes=[r_rm])
    P.op("dve", lambda e: [e.memset(ones[:], 1.0)], writes=[r_ones])
    for h2 in range(8):
        st.dma(g2f[:, :, :], g2_d[:, 2 * h2:2 * h2 + 2, :], writes=[r_g2f], res=r_g2f)
        P.op("dve", lambda e, h2=h2: [e.tensor_copy(out=g2[:, 2 * h2:2 * h2 + 2, :], in_=g2f[:, :, :])], reads=[r_g2f], writes=[r_g2])
    si = 0
    oi = 0
    pi = 0
    its = []
    pool_parts = {}
    for B in range(NBLK):
        b0 = 2 * B
        ks0 = ks_of(b0)
        st0 = (ks0 + 4) * 64
        mb = B % 2

        def pool_part(B=B, st0=st0, mb=mb):
            if B >= NBLK:
                return
            st.dma(aT[:, :, :], AT_d.rearrange("c p n -> p c n")[:, :, st0 - 16:st0 + 528], writes=[r_aT], res=r_aT)
            st.dma(invc[:, :, :], invc_d[:, B * 512:(B + 1) * 512].partition_broadcast(128), writes=[r_invc], res=r_invc)
            for c in range(8):
                g = c // 2

                def pool_ops(e, c=c, g=g):
                    ins = [e.tensor_tensor(out=T[:, 0, 1:544], in0=aT[:, c, 0:543], in1=aT[:, c, 1:544], op=ALU.add)]
                    if g >= 1:
                        ins.append(e.tensor_tensor(out=T[:, 1, 2:543], in0=T[:, 0, 1:542], in1=T[:, 0, 3:544], op=ALU.add))
                    if g >= 2:
                        ins.append(e.tensor_tensor(out=T[:, 2, 4:541], in0=T[:, 1, 2:539], in1=T[:, 1, 6:543], op=ALU.add))
                    if g >= 3:
                        ins.append(e.tensor_tensor(out=T[:, 3, 8:537], in0=T[:, 2, 4:533], in1=T[:, 2, 12:541], op=ALU.add))
                    ins.append(e.tensor_tensor(out=T[:, g, 16:528], in0=T[:, g, 16:528], in1=invc[:, g, :], op=ALU.mult))
                    ins.append(e.tensor_tensor(out=pT[:, c, :], in0=T[:, g, 16:528], in1=aT[:, c, 16:528], op=ALU.subtract))
                    return ins
                n_ins = 3 + g
                for ii in range(n_ins):
                    P.op("pool", lambda e, c=c, g=g, ii=ii, f=pool_ops: [_nth(f, e, ii)], reads=[r_aT, r_invc, r_T],
                         writes=[r_T] + ([r_pT[c]] if ii == n_ins - 1 else []))
            for dc in range(8):
                g, dh = dc // 2, dc % 2
                P.op("pe", lambda e, g=g, dh=dh: [e.matmul(ps_p[:, :], lhsT=wp[:, g, cc, dh * 128:(dh + 1) * 128], rhs=pT[:, 2 * g + cc, :],
                                                            start=(cc == 0), stop=(cc == 1)) for cc in range(2)],
                     reads=[r_wp, r_pT[2 * g], r_pT[2 * g + 1]], writes=[r_psp])
                P.op("act", lambda e, dc=dc, mb=mb: [e.activation(out=mixT[:, mb, dc, :], in_=ps_p[:, :], func=AF.Copy, scale=psc[:, dc:dc + 1])],
                     reads=[r_psp, r_psc], writes=[r_mix[mb]])
        pool_parts[B] = pool_part
        for hp in range(8):
            q = hp % 2

            def load_hp(hp=hp, q=q, ks0=ks0, st0=st0):
                P.op("sp", lambda e: [
                    e.dma_start(out=KT[:, q, :], in_=KT_d[hp, :, ks0 * 64:ks0 * 64 + 1024]),
                    e.dma_start(out=QT[:, q, :], in_=QT_d[hp, :, st0:st0 + 512]),
                    e.dma_start(out=V[:, q, :, :], in_=V_d[ks0 * 64:ks0 * 64 + 1024, hp * 128:(hp + 1) * 128].rearrange("(t p) f -> p t f", p=128))],
                     writes=[r_hp[q]], dma_res=r_hp[q], n_dma=3)
            for bi in range(2):
                b = b0 + bi
                o = oi % 2
                oi += 1
                for pr in range(6):
                    s = si % 3
                    si += 1
                    koff = bi * 256 + pr * 128
                    vt = bi * 2 + pr
                    p = pi % 4
                    pi += 1
                    pre = []
                    if B == 0 and hp == 0 and bi == 0 and pr == 0:
                        pre.append(lambda: pool_parts[0]())
                    if hp == 4 and bi == 0 and pr == 0 and B + 1 < NBLK:
                        pre.append(lambda B=B: pool_parts[B + 1]())
                    if bi == 0 and pr == 0:
                        pre.append(load_hp)

                    def front(hp=hp, q=q, bi=bi, b=b, pr=pr, s=s, koff=koff, p=p):
                        def smm(e):
                            ins = []
                            for hh in range(2):
                                lo, hi = hh * 64, hh * 64 + 64
                                h = 2 * hp + hh
                                out = ps_s[s][:, hh * 256:(hh + 1) * 256]
                                if _SUB.get("nobias"):
                                    ins.append(e.matmul(out, lhsT=KT[lo:hi, q, koff:koff + 128], rhs=QT[lo:hi, q, bi * 256:(bi + 1) * 256],
                                                        start=True, stop=True))
                                    continue
                                ins.append(e.matmul(out, lhsT=KT[lo:hi, q, koff:koff + 128], rhs=QT[lo:hi, q, bi * 256:(bi + 1) * 256],
                                                    start=True, stop=False))
                                ins.append(e.matmul(out, lhsT=idb[:, :], rhs=g2[:, h, (10 - 2 * pr) * 64:(10 - 2 * pr) * 64 + 256],
                                                    start=False, stop=False))
                                roff = (b * 6 + pr) * 4
                                ins.append(e.matmul(out, lhsT=e2[0:2, :], rhs=rm[0:2, roff:roff + 4].unsqueeze(2).broadcast_to([2, 4, 64]),
                                                    start=False, stop=True))
                            return ins
                        P.op("pe", smm, reads=[r_hp[q], r_idb, r_g2, r_e2, r_rm], writes=[r_pss[s]])
                        P.op("act", lambda e: [e.activation(out=PT[:, p, :], in_=ps_s[s][:, :], func=AF.Exp)],
                             reads=[r_pss[s]], writes=[r_PT[p]])

                    def back(hp=hp, q=q, bi=bi, vt=vt, p=p, o=o, pr=pr, mb=mb, B=B):
                        def pvmm(e):
                            ins = []
                            for hh in range(2):
                                lo, hi = hh * 64, hh * 64 + 64
                                ins.append(e.matmul(ps_o[o][lo:hi, 0:256], lhsT=V[:, q, vt, lo:hi], rhs=PT[:, p, hh * 256:(hh + 1) * 256],
                                                    start=(pr == 0), stop=(pr == 5)))
                                ins.append(e.matmul(ps_d[o][lo:hi, 0:256], lhsT=ones[:, 0:64], rhs=PT[:, p, hh * 256:(hh + 1) * 256],
                                                    start=(pr == 0), stop=(pr == 5)))
                            return ins
                        P.op("pe", pvmm, reads=[r_hp[q], r_PT[p], r_ones], writes=[r_pso[o], r_psd[o]])
                        if pr == 5:
                            P.op("dve", lambda e: [e.reciprocal(out=rec[:, o, :], in_=ps_d[o][:, 0:256])], reads=[r_psd[o]], writes=[r_rec[o]])
                            P.op("dve", lambda e: [e.tensor_tensor(out=mixT[:, mb, 8 + hp, bi * 256:(bi + 1) * 256], in0=ps_o[o][:, 0:256],
                                                                   in1=rec[:, o, :], op=ALU.mult)],
                                 reads=[r_pso[o], r_rec[o]], writes=[r_mix[mb]])
                            if hp == 7 and bi == 1:
                                st.dma(mix_d.rearrange("c p n -> p c n")[:, :, B * 512:(B + 1) * 512], mixT[:, mb, :, :], reads=[r_mix[mb]], res=r_mix[mb])
                    its.append((pre, front, back))
    SK = _SUB.get('sk', 2)
    for idx in range(len(its) + SK):
        if idx < len(its):
            for f in its[idx][0]:
                f()
            its[idx][1]()
        if idx >= SK:
            its[idx - SK][2]()
    st.finish()


def _nth(f, e, ii):
    return _NthEngine(e, ii).run(f)


class _NthEngine:
    def __init__(self, e, ii):
        self.e, self.ii, self.n, self.res = e, ii, 0, None

    def __getattr__(self, name):
        def call(*a, **k):
            i = self.n
            self.n += 1
            if i == self.ii:
                self.res = getattr(self.e, name)(*a, **k)
                return self.res
            return None
        return call

    def run(self, f):
        f(self)
        return self.res


def stage_xattn(nc, QX_d, KTm_d, Vm_d, oT_d):
    st = Stage(nc, "s3c")
    P = st.P
    KTm = st.sb("KTm", [128, 16, 512], BF16)
    Vm = st.sb("Vm", [128, 4, D], BF16)
    ones = st.sb("ones", [128, 128], BF16)
    QX = st.sb("QX", [128, 2, 16, 512], BF16)
    PT = st.sb("PT", [128, 4, 512], BF16)
    rec = st.sb("rec", [128, 2, 512], F32)
    oT = st.sb("oT", [128, 2, 16, 512], BF16)
    ps_s = [st.ps() for _ in range(3)]
    ps_d = [st.ps() for _ in range(2)]
    ps_o = [st.ps() for _ in range(3)]
    r_k, r_v, r_ones = Res("k"), Res("v"), Res("ones")
    r_qx = [Res(f"qx{i}") for i in range(2)]
    r_oT = [Res(f"oT{i}") for i in range(2)]
    r_PT = [Res(f"PT{i}") for i in range(4)]
    r_rec = [Res(f"rec{i}") for i in range(2)]
    r_pss = [Res(f"pss{i}") for i in range(3)]
    r_psd = [Res(f"psd{i}") for i in range(2)]
    r_pso = [Res(f"pso{i}") for i in range(3)]
    st.dma(KTm[:, :, :], KTm_d.rearrange("c p n -> p c n"), writes=[r_k], res=r_k)
    st.dma(Vm[:, :, :], Vm_d.rearrange("(t p) f -> p t f", p=128), writes=[r_v], res=r_v)
    P.op("dve", lambda e: [e.memset(ones[:], 1.0)], writes=[r_ones])
    si = pi = di = oi = 0
    its = []
    for B in range(NBLK):
        par = B % 2
        seq = 0 if B < 4 else 1
        for h in range(4):
            ss, pts = [], []
            for mc in range(2):
                ss.append(si % 3)
                si += 1
                pts.append(pi % 4)
                pi += 1
            d = di % 2
            di += 1
            os_ = []
            for dk in range(4):
                os_.append(oi % 3)
                oi += 1

            def front(B=B, par=par, seq=seq, h=h, ss=tuple(ss), pts=tuple(pts)):
                if h == 0:
                    st.dma(QX[:, par, :, :], QX_d.rearrange("c p n -> p c n")[:, :, B * 512:(B + 1) * 512], writes=[r_qx[par]], res=r_qx[par])
                for mc in range(2):
                    s, p = ss[mc], pts[mc]
                    P.op("pe", lambda e, mc=mc, s=s: [
                        e.matmul(ps_s[s][:, :], lhsT=KTm[:, 4 * h + dk, seq * 256 + mc * 128:seq * 256 + (mc + 1) * 128],
                                 rhs=QX[:, par, 4 * h + dk, :], start=(dk == 0), stop=(dk == 3)) for dk in range(4)],
                         reads=[r_k, r_qx[par]], writes=[r_pss[s]])
                    P.op("act", lambda e, s=s, p=p: [e.activation(out=PT[:, p, :], in_=ps_s[s][:, :], func=AF.Exp)],
                         reads=[r_pss[s]], writes=[r_PT[p]])

            def back(B=B, par=par, seq=seq, h=h, pts=tuple(pts), d=d, os_=tuple(os_)):
                P.op("pe", lambda e: [e.matmul(ps_d[d][:, :], lhsT=ones[:, :], rhs=PT[:, pts[mc], :], start=(mc == 0), stop=(mc == 1))
                                      for mc in range(2)], reads=[r_ones, r_PT[pts[0]], r_PT[pts[1]]], writes=[r_psd[d]])
                P.op("dve", lambda e: [e.reciprocal(out=rec[:, d, :], in_=ps_d[d][:, :])], reads=[r_psd[d]], writes=[r_rec[d]])
                for dk in range(4):
                    o = os_[dk]
                    P.op("pe", lambda e, o=o, dk=dk: [
                        e.matmul(ps_o[o][:, :], lhsT=Vm[:, seq * 2 + mc, (4 * h + dk) * 128:(4 * h + dk + 1) * 128], rhs=PT[:, pts[mc], :],
                                 start=(mc == 0), stop=(mc == 1)) for mc in range(2)],
                         reads=[r_v, r_PT[pts[0]], r_PT[pts[1]]], writes=[r_pso[o]])
                    P.op("dve", lambda e, o=o, dk=dk: [e.tensor_tensor(out=oT[:, par, 4 * h + dk, :], in0=ps_o[o][:, :],
                                                                       in1=rec[:, d, :], op=ALU.mult)],
                         reads=[r_pso[o], r_rec[d]], writes=[r_oT[par]])
                if h == 3:
                    st.dma(oT_d.rearrange("c p n -> p c n")[:, :, B * 512:(B + 1) * 512], oT[:, par, :, :], reads=[r_oT[par]], res=r_oT[par])
            its.append((front, back))
    SK = 1
    for idx in range(len(its) + SK):
        if idx < len(its):
            its[idx][0]()
        if idx >= SK:
            its[idx - SK][1]()
    st.finish()


def stage_router(nc, x2_d, wr_d, brep_d, ident_d, triu_d, iotaE_d, tokid_d, tab_d, tabinit_d, sidx_d, GT_d, yzero_d, Y_d, x2b_d):
    st = Stage(nc, "s4")
    P = st.P
    wr = st.sb("wr", [128, 16, NE], F32)
    brep = st.sb("brep", [128, NE], F32)
    idf = st.sb("idf", [128, 128], F32)
    triu = st.sb("triu", [128, 128], F32)
    triub = st.sb("triub", [128, 128], BF16)
    onesb = st.sb("onesb", [128, 128], BF16)
    iotaE = st.sb("iotaE", [128, NE], F32)
    tokid = st.sb("tokid", [128, 24], F32)
    carry = st.sb("carry", [128, NE], F32)
    tinit = st.sb("tinit", [128, NSLOT // 128 + 1, 2], F32)
    zrow = st.sb("zrow", [1, D], BF16)
    xt = st.sb("xt", [128, 2, D], F32)
    xT = st.sb("xT", [128, 2, 16, 128], F32)
    lg = st.sb("lg", [128, 2, 8, NE], F32)
    Ab = st.sb("Ab", [128, 2, NE], BF16)
    m8 = st.sb("m8", [128, 2, 32], F32)
    si4 = st.sb("si4", [128, 2, 4], I32)
    sc = st.sb("sc", [128, 2, 4, 2], F32)
    GT = st.sb("GT", [32, 2, 128], F32)
    ps_t = [st.ps() for _ in range(2)]
    ps_l = [st.ps() for _ in range(2)]
    ps_p = [st.ps() for _ in range(2)]
    ps_g = [st.ps() for _ in range(2)]
    R = lambda n: Res(n)
    r_wr, r_brep, r_idf, r_triu, r_triub, r_onesb, r_iotaE, r_tokid, r_carry, r_tinit, r_tab, r_zrow = (R(n) for n in (
        "wr", "brep", "idf", "triu", "triub", "onesb", "iotaE", "tokid", "carry", "tinit", "tab", "zrow"))
    r_xt = [R(f"xt{i}") for i in range(2)]
    r_xT = [R(f"xT{i}") for i in range(2)]
    r_w = [R(f"w{i}") for i in range(2)]
    r_sc = [R(f"sc{i}") for i in range(2)]
    r_si = [R(f"si{i}") for i in range(2)]
    r_GT = [R(f"GT{i}") for i in range(2)]
    r_pst = [R(f"pst{i}") for i in range(2)]
    r_psl = [R(f"psl{i}") for i in range(2)]
    r_psp = [R(f"psp{i}") for i in range(2)]
    r_psg = [R(f"psg{i}") for i in range(2)]
    st.dma(wr[:, :, :], wr_d.rearrange("(k p) n -> p k n", p=128), writes=[r_wr], res=r_wr)
    st.dma(brep[:], brep_d, writes=[r_brep], res=r_brep)
    st.dma(idf[:], ident_d, writes=[r_idf], res=r_idf)
    st.dma(triu[:], triu_d, writes=[r_triu], res=r_triu)
    st.dma(iotaE[:], iotaE_d, writes=[r_iotaE], res=r_iotaE)
    st.dma(tokid[:], tokid_d, writes=[r_tokid], res=r_tokid)
    st.dma(tinit[:, :, :], tabinit_d, writes=[r_tinit], res=r_tinit)
    P.op("dve", lambda e: [e.tensor_copy(out=triub[:], in_=triu[:])], reads=[r_triu], writes=[r_triub])
    P.op("dve", lambda e: [e.memset(onesb[:], 1.0)], writes=[r_onesb])
    P.op("dve", lambda e: [e.memset(carry[:], 0.0)], writes=[r_carry])
    P.op("dve", lambda e: [e.memset(zrow[:], 0.0)], writes=[r_zrow])
    st.dma(tab_d[0:NSLOT, :].rearrange("(p t) c -> p t c", p=128), tinit[:, 0:NSLOT // 128, :], reads=[r_tinit], writes=[r_tab], res=r_tinit)
    st.dma(tab_d[NSLOT:NSLOT + 1, :], tinit[0:1, NSLOT // 128, :], reads=[r_tinit], writes=[r_tab], res=r_tinit)
    st.dma(Y_d[0:1, :], zrow[:, :], reads=[r_zrow], res=r_zrow)
    st.dma(x2b_d[NOWN:NOWN + 1, :], zrow[:, :], reads=[r_zrow], res=r_zrow)
    def part(ti, which):
        q = ti % 2
        L = lambda pl, q=q: lg[:, q, pl, :]
        M = lambda a, b2, q=q: m8[:, q, a:b2]
        if which == "B":
            return partB(ti, q, L, M)
        st.dma(xt[:, q, :], x2_d[ti * 128:(ti + 1) * 128, :], writes=[r_xt[q]], res=r_xt[q])
        for kk in range(4):
            b = kk % 2
            P.op("pe", lambda e, kk=kk, b=b, q=q: [e.transpose(out=ps_t[b][:, j * 128:(j + 1) * 128],
                                                               in_=xt[:, q, (4 * kk + j) * 128:(4 * kk + j + 1) * 128], identity=idf[:])
                                                   for j in range(4)], reads=[r_idf, r_xt[q]], writes=[r_pst[b]])
            st.evac(xT[:, q, 4 * kk:4 * kk + 4, :], ps_t[b][:, :].rearrange("p (j n) -> p j n", j=4), reads=[r_pst[b]], writes=[r_xT[q]])
        P.op("pe", lambda e, q=q: [e.matmul(ps_l[q][:, 0:NE], lhsT=xT[:, q, k, :], rhs=wr[:, k, :], start=(k == 0), stop=(k == 15))
                                   for k in range(16)], reads=[r_wr, r_xT[q]], writes=[r_psl[q]])
        W = [r_w[q]]

        def dv(f, reads=(), writes=None, q=q):
            P.op("dve", f, reads=list(reads) + [r_w[q]], writes=[r_w[q]] if writes is None else writes)
        dv(lambda e, q=q, L=L: [e.tensor_tensor(out=L(0), in0=ps_l[q][:, 0:NE], in1=brep[:], op=ALU.add)], reads=[r_psl[q], r_brep])
        dv(lambda e, L=L, M=M: [e.max(out=M(0, 8), in_=L(0))])
        dv(lambda e, L=L, M=M: [e.tensor_scalar(out=L(1), in0=L(0), scalar1=M(3, 4), scalar2=None, op0=ALU.is_ge)])
        dv(lambda e, M=M: [e.tensor_scalar(out=M(16, 17), in0=M(0, 1), scalar1=-1.0, scalar2=None, op0=ALU.mult)])
        P.op("act", lambda e, L=L, M=M: [e.activation(out=L(2), in_=L(0), func=AF.Exp, bias=M(16, 17), scale=1.0)], reads=W, writes=W)
        dv(lambda e, L=L, M=M: [e.tensor_tensor(out=L(3), in0=L(2), in1=L(1), op=ALU.mult)])
        dv(lambda e, L=L, M=M: [e.tensor_reduce(out=M(17, 18), in_=L(3), axis=mybir.AxisListType.X, op=ALU.add)])
        dv(lambda e, M=M: [e.reciprocal(out=M(18, 19), in_=M(17, 18))])
        dv(lambda e, L=L, M=M: [e.tensor_scalar(out=L(3), in0=L(3), scalar1=M(18, 19), scalar2=None, op0=ALU.mult)])
        dv(lambda e, L=L, q=q: [e.tensor_copy(out=Ab[:, q, :], in_=L(1))])

    def partB(ti, q, L, M):
        W = [r_w[q]]

        def dv(f, reads=(), writes=None, q=q):
            P.op("dve", f, reads=list(reads) + [r_w[q]], writes=[r_w[q]] if writes is None else writes)
        P.op("pe", lambda e, q=q: [e.matmul(ps_p[q][:, 0:NE], lhsT=triub[:, :], rhs=Ab[:, q, :], start=True, stop=True),
                                   e.matmul(ps_p[q][:, 64:64 + NE], lhsT=onesb[:, :], rhs=Ab[:, q, :], start=True, stop=True)],
             reads=[r_triub, r_onesb, r_w[q]], writes=[r_psp[q]])
        dv(lambda e, L=L, q=q: [e.tensor_tensor(out=L(4), in0=ps_p[q][:, 0:NE], in1=carry[:], op=ALU.add)], reads=[r_psp[q], r_carry])
        P.op("dve", lambda e, q=q: [e.tensor_tensor(out=carry[:], in0=carry[:], in1=ps_p[q][:, 64:64 + NE], op=ALU.add)],
             reads=[r_psp[q], r_carry, r_w[q]], writes=[r_carry])
        dv(lambda e, L=L: [e.tensor_scalar(out=L(5), in0=L(4), scalar1=float(CAP), scalar2=None, op0=ALU.is_lt)])
        dv(lambda e, L=L: [e.tensor_tensor(out=L(5), in0=L(5), in1=L(1), op=ALU.mult)])
        dv(lambda e, L=L: [e.tensor_tensor(out=L(6), in0=L(4), in1=iotaE[:], op=ALU.add)], reads=[r_iotaE])
        dv(lambda e, L=L: [e.tensor_tensor(out=L(6), in0=L(6), in1=L(5), op=ALU.mult)])
        dv(lambda e, L=L, M=M: [e.max(out=M(8, 16), in_=L(6))])
        for k in range(4):
            dv(lambda e, L=L, M=M, k=k: [e.tensor_scalar(out=L(7), in0=L(6), scalar1=M(8 + k, 9 + k), scalar2=None, op0=ALU.is_equal)])
            dv(lambda e, L=L: [e.tensor_tensor(out=L(7), in0=L(7), in1=L(3), op=ALU.mult)])
            dv(lambda e, L=L, M=M, k=k: [e.tensor_reduce(out=M(20 + k, 21 + k), in_=L(7), axis=mybir.AxisListType.X, op=ALU.add)])
        P.op("dve", lambda e, q=q, M=M: [e.tensor_copy(out=si4[:, q, :], in_=M(8, 12))], reads=[r_w[q]], writes=[r_si[q]])
        P.op("dve", lambda e, q=q, M=M, ti=ti: [e.tensor_copy(out=sc[:, q, :, 1], in_=M(20, 24))], reads=[r_w[q]], writes=[r_sc[q]])
        P.op("dve", lambda e, q=q, ti=ti: [e.tensor_copy(out=sc[:, q, :, 0], in_=tokid[:, ti:ti + 1].broadcast_to([128, 4]))],
             reads=[r_tokid, r_sc[q]], writes=[r_sc[q]])
        for k in range(4):
            P.op("pool", lambda e, q=q, k=k: [e.indirect_dma_start(out=tab_d[:, :], out_offset=bass.IndirectOffsetOnAxis(ap=si4[:, q, k:k + 1], axis=0),
                                                                   in_=sc[:, q, k, :], in_offset=None)],
                 reads=[r_si[q], r_sc[q], r_tab], writes=[r_tab], dma_res=r_sc[q], n_dma=1)
        st.dma(sidx_d[ti], si4[:, q, :], reads=[r_si[q]], res=r_si[q], eng="pool")
        P.op("pe", lambda e, q=q, L=L: [e.transpose(out=ps_g[q][0:NE, 0:128], in_=L(3), identity=idf[:])], reads=[r_w[q], r_idf], writes=[r_psg[q]])
        P.op("act", lambda e, q=q: [e.activation(out=GT[:, q, :], in_=ps_g[q][0:NE, 0:128], func=AF.Copy)], reads=[r_psg[q]], writes=[r_GT[q]])
        st.dma(GT_d[:, ti * 128:(ti + 1) * 128], GT[:, q, :], reads=[r_GT[q]], res=r_GT[q], eng="pool")

    for idx in range(25):
        if idx < 24:
            part(idx, "A")
        if idx >= 1:
            part(idx - 1, "B")
    st.finish()


def stage_experts(nc, tab_d, x2b_d, w_gu, w_down, bgu_d, Y_d, ident_d, n_exp=NE):
    st = Stage(nc, "s5")
    P = st.P
    idf = st.sb("idf", [128, 128], F32)
    idb = st.sb("idb", [128, 128], BF16)
    bgu = st.sb("bgu", [128, NE, 32], F32)
    bgu1 = st.sb("bgu1", [128, NE, 16], F32)
    tb = st.sb("tb", [128, 2, 4, 2], F32)
    ti4 = st.sb("ti4", [128, 2, 4], I32)
    xg = st.sb("xg", [128, 4, D], BF16)
    XT = st.sb("XT", [128, 16, 512], BF16)
    S = st.sb("S", [128, 4, 16 * 256], F32)
    W = st.sb("W", [128, 3, 16, 2, 256], BF16)
    actT = st.sb("actT", [128, 16, 512], BF16)
    Y = st.sb("Y", [128, 4, D], BF16)
    tmp = st.sb("tmp", [128, 2, 4, 512], F32)
    ps_t = [st.ps(BF16, 1024) for _ in range(2)]
    ps_g = [st.ps() for _ in range(2)]
    ps_u = [st.ps() for _ in range(2)]
    ps_y = [st.ps() for _ in range(2)]
    R = lambda n: Res(n)
    r_idf, r_idb, r_bgu, r_XT, r_actT, r_Y = R("idf"), R("idb"), R("bgu"), R("XT"), R("actT"), R("Y")
    r_tb = [R(f"tb{i}") for i in range(2)]
    r_ti = [R(f"ti{i}") for i in range(2)]
    r_xg = [R(f"xg{i}") for i in range(4)]
    r_S = [R(f"S{i}") for i in range(4)]
    r_W = [[R(f"W{i}_{h}") for h in range(2)] for i in range(3)]
    r_tmp = [R(f"tmp{i}") for i in range(2)]
    r_pst = [R(f"pst{i}") for i in range(2)]
    r_psg = [R(f"psg{i}") for i in range(2)]
    r_psu = [R(f"psu{i}") for i in range(2)]
    r_psy = [R(f"psy{i}") for i in range(2)]
    pairs = [(ex, kind, i) for ex in range(n_exp) for (kind, cnt) in (("gu", 8), ("d", 4)) for i in range(cnt)]
    NP = len(pairs)
    CAST_ENG = ("act", "dve", "act", "pool", "act", "dve", "act", "act", "dve", "act", "pool", "act")

    def src(pr, h):
        ex, kind, i = pr
        if kind == "gu":
            return w_gu[ex].rearrange("(k p) n -> p k n", p=128)[:, :, h * 2048 + i * 256:h * 2048 + (i + 1) * 256]
        return w_down[ex].rearrange("(k p) n -> p k n", p=128)[:, :, i * 512 + h * 256:i * 512 + (h + 1) * 256]

    def load(pi):
        if _SUB.get("noload") and pi >= 4:
            return
        for h in range(2):
            si = (pi % 2) * 2 + h
            st.dma(S[:, si, :].rearrange("p (k n) -> p k n", k=16), src(pairs[pi], h), writes=[r_S[si]], res=r_S[si], eng="sp")

    def cast(pi):
        for h in range(2):
            si, wi = (pi % 2) * 2 + h, pi % 3
            eng = CAST_ENG[(2 * pi + h) % 12]
            outs = [W[:, wi, 8 * i:8 * i + 8, h, :] for i in range(2)]
            ins = [S[:, si, 2048 * i:2048 * (i + 1)].rearrange("p (k n) -> p k n", k=8) for i in range(2)]
            if eng == "act":
                P.op("act", lambda e, outs=outs, ins=ins: [e.activation(out=o, in_=i_, func=AF.Copy) for o, i_ in zip(outs, ins)],
                     reads=[r_S[si]], writes=[r_W[wi][h]])
            else:
                P.op(eng, lambda e, outs=outs, ins=ins: [e.tensor_copy(out=o, in_=i_) for o, i_ in zip(outs, ins)],
                     reads=[r_S[si]], writes=[r_W[wi][h]])

    def prep_tokens(ex):
        q = ex % 2
        s0 = ex * CAP + 1
        st.dma(tb[:, q, :, :], tab_d[s0:s0 + CAP, :].rearrange("(m p) c -> p m c", p=128), writes=[r_tb[q]], res=r_tb[q], eng="act")
        P.op("dve", lambda e, q=q: [e.tensor_copy(out=ti4[:, q, :], in_=tb[:, q, :, 0])], reads=[r_tb[q]], writes=[r_ti[q]])
        for m in range(4):
            P.op("pool", lambda e, q=q, m=m: [e.indirect_dma_start(out=xg[:, m, :], out_offset=None, in_=x2b_d[:, :],
                                                                   in_offset=bass.IndirectOffsetOnAxis(ap=ti4[:, q, m:m + 1], axis=0))],
                 reads=[r_ti[q]], writes=[r_xg[m]], dma_res=r_xg[m], n_dma=1)

    def do_transposes(ex):
        for k2 in range(8):
            b = k2 % 2
            P.op("pe", lambda e, k2=k2, b=b: [e.transpose(out=ps_t[b][:, (kk * 4 + m) * 128:(kk * 4 + m + 1) * 128],
                                                          in_=xg[:, m, (2 * k2 + kk) * 128:(2 * k2 + kk + 1) * 128], identity=idb[:])
                                              for kk in range(2) for m in range(4)], reads=[r_idb] + r_xg, writes=[r_pst[b]])
            st.evac(XT[:, 2 * k2:2 * k2 + 2, :], ps_t[b][:, :].rearrange("p (k n) -> p k n", k=2), reads=[r_pst[b]], writes=[r_XT])

    def compute_gu(ex, j2, wi):
        for jj in range(2):
            j = 2 * j2 + jj
            pb = j % 2
            P.op("pe", lambda e, wi=wi, jj=jj, pb=pb: [e.matmul(ps_g[pb][:, :], lhsT=W[:, wi, k, 0, jj * 128:(jj + 1) * 128], rhs=XT[:, k, :],
                                                                start=(k == 0), stop=(k == 15)) for k in range(16)],
                 reads=[r_W[wi][0], r_XT], writes=[r_psg[pb]])
            P.op("pe", lambda e, wi=wi, jj=jj, pb=pb: [e.matmul(ps_u[pb][:, :], lhsT=W[:, wi, k, 1, jj * 128:(jj + 1) * 128], rhs=XT[:, k, :],
                                                                start=(k == 0), stop=(k == 15)) for k in range(16)],
                 reads=[r_W[wi][1], r_XT], writes=[r_psu[pb]])
            T = lambda i, pb=pb: tmp[:, pb, i, :]
            rt = [r_tmp[pb]]
            P.op("dve", lambda e, T=T, pb=pb, ex=ex, j=j: [e.tensor_scalar(out=T(0), in0=ps_g[pb][:, :], scalar1=bgu[:, ex, j:j + 1], scalar2=7.0,
                                                                            op0=ALU.add, op1=ALU.min)], reads=[r_psg[pb], r_bgu] + rt, writes=rt)
            P.op("act", lambda e, T=T: [e.activation(out=T(1), in_=T(0), func=AF.Sigmoid, scale=1.702)], reads=rt, writes=rt)
            P.op("dve", lambda e, T=T, pb=pb, ex=ex, j=j: [e.tensor_scalar(out=T(2), in0=ps_u[pb][:, :], scalar1=bgu1[:, ex, j:j + 1], scalar2=8.0,
                                                                            op0=ALU.add, op1=ALU.min)], reads=[r_psu[pb], r_bgu] + rt, writes=rt)
            P.op("pool", lambda e, T=T: [e.tensor_tensor(out=T(3), in0=T(0), in1=T(1), op=ALU.mult)], reads=rt, writes=rt)
            P.op("dve", lambda e, T=T, j=j: [e.scalar_tensor_tensor(out=actT[:, j, :], in0=T(2), scalar=-6.0, in1=T(3), op0=ALU.max, op1=ALU.mult)],
                 reads=rt, writes=rt + [r_actT])

    def compute_d(ex, n, wi):
        q = ex % 2
        for m in range(4):
            pb = m % 2
            P.op("pe", lambda e, wi=wi, m=m, pb=pb: [e.matmul(ps_y[pb][:, :], lhsT=actT[:, k, m * 128:(m + 1) * 128],
                                                              rhs=W[:, wi, k, :, :].rearrange("p h n -> p (h n)"), start=(k == 0), stop=(k == 15))
                                                     for k in range(16)],
                 reads=[r_W[wi][0], r_W[wi][1], r_actT], writes=[r_psy[pb]])
            P.op("act", lambda e, m=m, n=n, pb=pb, q=q: [e.activation(out=Y[:, m, n * 512:(n + 1) * 512], in_=ps_y[pb][:, :], func=AF.Copy,
                                                                      scale=tb[:, q, m, 1:2])], reads=[r_psy[pb], r_tb[q]], writes=[r_Y])

    load(0)
    load(1)
    st.dma(idf[:], ident_d, writes=[r_idf], res=r_idf, eng="act")
    st.dma(bgu[:, :, :], bgu_d, writes=[r_bgu], res=r_bgu, eng="act")
    P.op("dve", lambda e: [e.tensor_copy(out=idb[:], in_=idf[:])], reads=[r_idf], writes=[r_idb])
    P.op("dve", lambda e: [e.tensor_scalar(out=bgu1[:, :, :], in0=bgu[:, :, 16:32], scalar1=1.0, scalar2=None, op0=ALU.add)], reads=[r_bgu], writes=[r_bgu])
    prep_tokens(0)
    cast(0)
    load(2)
    cast(1)
    load(3)
    do_transposes(0)
    for pi, (ex, kind, i) in enumerate(pairs):
        if pi + 2 < NP:
            cast(pi + 2)
        if pi + 4 < NP:
            load(pi + 4)
        if kind == "gu":
            compute_gu(ex, i, pi % 3)
            if i == 1 and ex + 1 < n_exp:
                prep_tokens(ex + 1)
            if i == 7 and ex + 1 < n_exp:
                do_transposes(ex + 1)
        else:
            compute_d(ex, i, pi % 3)
            if i == 3:
                s0 = ex * CAP + 1
                st.dma(Y_d[s0:s0 + CAP, :].rearrange("(m p) d -> p m d", p=128), Y[:, :, :], reads=[r_Y], res=r_Y, eng="act")
    st.finish()


def stage_combine(nc, x2_d, Y_d, sidx_d, GT_d, bdown_d, gam_d, bet_d, out_d):
    st = Stage(nc, "s6")
    P = st.P
    NB = 3
    gam = st.sb("gam", [128, D], F32)
    bet = st.sb("bet", [128, D], F32)
    bd = st.sb("bd", [32, D], F32)
    si4 = st.sb("si4", [128, NB, 4], I32)
    GT = st.sb("GT", [32, NB, 128], F32)
    xr = st.sb("xr", [128, NB, D], F32)
    y = st.sb("y", [128, NB, D], F32)
    Yg = st.sb("Yg", [128, NB, 4, D], BF16)
    stats = st.sb("stats", [128, NB, 4, 6], F32)
    mv = st.sb("mv", [128, NB, 4], F32)
    ps = [st.ps() for _ in range(8)]
    R = lambda n: Res(n)
    r_g, r_b, r_bd = R("g"), R("b"), R("bd")
    r_si = [R(f"si{i}") for i in range(NB)]
    r_GT = [R(f"GT{i}") for i in range(NB)]
    r_xr = [R(f"xr{i}") for i in range(NB)]
    r_y = [R(f"y{i}") for i in range(NB)]
    r_Yg = [[R(f"Yg{i}_{k}") for k in range(4)] for i in range(NB)]
    r_st = [R(f"st{i}") for i in range(NB)]
    r_ps = [R(f"ps{i}") for i in range(8)]
    st.dma(gam[:], gam_d, writes=[r_g], res=r_g)
    st.dma(bet[:], bet_d, writes=[r_b], res=r_b)
    st.dma(bd[:, :], bdown_d, writes=[r_bd], res=r_bd)
    for ti in range(24):
        q = ti % NB
        pq = ti % 2
        r0 = ti * 128
        st.dma(si4[:, q, :], sidx_d[ti], writes=[r_si[q]], res=r_si[q])
        st.dma(GT[:, q, :], GT_d[:, r0:r0 + 128], writes=[r_GT[q]], res=r_GT[q])
        st.dma(xr[:, q, :], x2_d[r0:r0 + 128, :], writes=[r_xr[q]], res=r_xr[q])
        for k in range(4):
            P.op("pool", lambda e, q=q, k=k: [e.indirect_dma_start(out=Yg[:, q, k, :], out_offset=None, in_=Y_d[:, :],
                                                                   in_offset=bass.IndirectOffsetOnAxis(ap=si4[:, q, k:k + 1], axis=0))],
                 reads=[r_si[q]], writes=[r_Yg[q][k]], dma_res=r_Yg[q][k], n_dma=1)
        for n in range(4):
            b = pq * 4 + n
            P.op("pe", lambda e, q=q, n=n, b=b: [e.matmul(ps[b][:, :], lhsT=GT[:, q, :], rhs=bd[:, n * 512:(n + 1) * 512], start=True, stop=True)],
                 reads=[r_GT[q], r_bd], writes=[r_ps[b]])
            P.op("dve", lambda e, n=n, b=b, q=q: [e.scalar_tensor_tensor(out=y[:, q, n * 512:(n + 1) * 512], in0=xr[:, q, n * 512:(n + 1) * 512],
                                                                         scalar=ALPHA, in1=ps[b][:, :], op0=ALU.mult, op1=ALU.add)],
                 reads=[r_xr[q], r_ps[b]], writes=[r_y[q]])
        for k in range(4):
            P.op("dve", lambda e, q=q, k=k: [e.tensor_tensor(out=y[:, q, :], in0=y[:, q, :], in1=Yg[:, q, k, :], op=ALU.add)],
                 reads=[r_y[q], r_Yg[q][k]], writes=[r_y[q]])
        ln_tail(st, y[:, q, :], stats[:, q, :, :], mv[:, q, :], gam, bet, r_y[q], r_st[q], [r_g, r_b])
        st.dma(out_d[r0:r0 + 128, :], y[:, q, :], reads=[r_y[q]], res=r_y[q], eng="pool")
    st.finish()


def build_program(upto=99, n_exp=NE, dbg=(), only=None, feed=()):
    nc = bass.Bass("TRN2", target_bir_lowering=False)
    dt = nc.dram_tensor

    def inp(name, shape, dtype=F32):
        return dt(name, shape, dtype, kind="ExternalInput").ap()

    def scr(name, shape, dtype):
        kind = "ExternalInput" if name in feed else ("ExternalOutput" if name in dbg else "Internal")
        return dt(name, shape, dtype, kind=kind).ap()

    def run(k):
        return (k in only) if only is not None else (upto >= k)

    ident = inp("ident", [128, 128])
    x1_d = scr("x1_d", [NOWN, D], F32)
    if run(1) or run(2):
        xs = inp("xs", [NSLAB, D])
        w_in = inp("w_in", [D, 4096])
        AT_d = scr("AT_d", [8, 128, NSLAB], BF16)
        QT_d = scr("QT_d", [8, 128, NSLAB], BF16)
        KT_d = scr("KT_d", [8, 128, NSLAB], BF16)
        V_d = scr("V_d", [NSLAB, 1024], BF16)
    if run(1):
        stage_proj(nc, "s1a", xs, NSLAB, w_in, 0, 2048, [(0, 8, AT_d, None), (1024, 8, QT_d, 0.125)], [], ident)
        stage_proj(nc, "s1b", xs, NSLAB, w_in, 2048, 2048, [(0, 8, KT_d, None)], [(1024, 1024, V_d)], ident)
    if run(2):
        w_pool = inp("w_pool", [4, 256, 256])
        pscale = inp("pscale", [128, 8])
        g2 = inp("g2", [128, 16, 14 * 64])
        rm = inp("rm", [2, 288])
        e2 = inp("e2", [2, 128])
        invc = inp("invc", [4, NOWN])
        w_out = inp("w_out", [D, D])
        ln1g, ln1b = inp("ln1g", [128, D]), inp("ln1b", [128, D])
        xown = inp("xown", [NOWN, D])
        mix_d = scr("mix_d", [16, 128, NOWN], BF16)
        stage_mix(nc, AT_d, QT_d, KT_d, V_d, mix_d, w_pool, pscale, g2, rm, e2, invc, ident)
        stage_proj_ln(nc, "s2b", mix_d, w_out, xown, ln1g, ln1b, x1_d)
    x2_d = scr("x2_d", [NOWN, D], F32)
    x2b_d = scr("x2b_d", [NOWN + 1, D], BF16)
    if run(3):
        memc = inp("memc", [512, D])
        w_xq, w_xkv, w_xo = inp("w_xq", [D, D]), inp("w_xkv", [D, 4096]), inp("w_xo", [D, D])
        ln2g, ln2b = inp("ln2g", [128, D]), inp("ln2b", [128, D])
        KTm_d = scr("KTm_d", [16, 128, 512], BF16)
        Vm_d = scr("Vm_d", [512, D], BF16)
        QX_d = scr("QX_d", [16, 128, NOWN], BF16)
        oT_d = scr("oT_d", [16, 128, NOWN], BF16)
        sub = only_sub if (only_sub := _SUB.get("s3")) else ("k", "v", "q", "x", "d")
        if "k" in sub:
            stage_proj(nc, "s3k", memc, 512, w_xkv, 0, 2048, [(0, 16, KTm_d, None)], [], ident)
        if "v" in sub:
            stage_proj(nc, "s3v", memc, 512, w_xkv, 2048, 2048, [], [(0, 2048, Vm_d)], ident)
        if "q" in sub:
            stage_proj(nc, "s3q", x1_d, NOWN, w_xq, 0, 2048, [(0, 16, QX_d, 512.0 ** -0.5)], [], ident)
        if "x" in sub:
            stage_xattn(nc, QX_d, KTm_d, Vm_d, oT_d)
        if "d" in sub:
            stage_proj_ln(nc, "s3d", oT_d, w_xo, x1_d, ln2g, ln2b, x2_d, out16=x2b_d)
    tab_d = scr("tab_d", [NSLOT + 1, 2], F32)
    sidx_d = scr("sidx_d", [24, 128, 4], I32)
    GT_d = scr("GT_d", [NE, NOWN], F32)
    Y_d = scr("Y_d", [NSLOT + 1, D], BF16)
    if run(4):
        w_router = inp("w_router", [D, NE])
        brep = inp("brep", [128, NE])
        triu = inp("triu", [128, 128])
        iotaE = inp("iotaE", [128, NE])
        tokid = inp("tokid", [128, 24])
        tabinit = inp("tabinit", [128, NSLOT // 128 + 1, 2])
        stage_router(nc, x2_d, w_router, brep, ident, triu, iotaE, tokid, tab_d, tabinit, sidx_d, GT_d, None, Y_d, x2b_d)
    if run(5):
        w_gu = inp("w_gu", [n_exp, D, 4096])
        w_down = inp("w_down", [n_exp, D, D])
        bgu = inp("bgu", [128, NE, 32])
        stage_experts(nc, tab_d, x2b_d, w_gu, w_down, bgu, Y_d, ident, n_exp=n_exp)
    if run(6):
        bdown = inp("bdown", [NE, D])
        ln3g, ln3b = inp("ln3g", [128, D]), inp("ln3b", [128, D])
        out_d = dt("out", [NOWN, D], F32, kind="ExternalOutput").ap()
        stage_combine(nc, x2_d, Y_d, sidx_d, GT_d, bdown, ln3g, ln3b, out_d)
    return nc


_SUB = {}


def _rep(v):
    return np.ascontiguousarray(np.broadcast_to(np.asarray(v, np.float32)[None, :], (128, v.shape[0])))


def make_inputs(inp, upto=99, n_exp=NE):
    f = lambda k: np.asarray(inp[k], np.float32)[0]
    x_p, x_s = np.asarray(inp["x_prompt"], np.float32), np.asarray(inp["x_sample"], np.float32)
    mem_p, mem_s = np.asarray(inp["mem_prompt"], np.float32), np.asarray(inp["mem_sample"], np.float32)
    rpb = f("rpb")
    kc = np.arange(64)
    qc = np.arange(64)
    cstart = np.clip(qc - 8, 0, 48)
    cvalid = (kc[:, None] >= cstart[None, :]) & (kc[:, None] < cstart[None, :] + 16)
    dcidx = np.clip(kc[:, None] - qc[None, :] + 15, 0, 30)
    g2 = np.empty((2, 64, 16, 14, 64), np.float32)
    for e in range(2):
        for Di in range(14):
            dr = (10 - Di) + 3 + e
            vals = rpb[:, dr, :][:, dcidx]
            g2[e, :, :, Di, :] = np.where(cvalid[None], vals, np.float32(-1e30)).transpose(1, 0, 2)
    g2 = np.ascontiguousarray(g2.reshape(128, 16, 14 * 64))
    e2 = np.zeros((2, 128), np.float32)
    e2[0, :64] = 1
    e2[1, 64:] = 1
    ident = np.eye(128, dtype=np.float32)
    triu = np.triu(np.ones((128, 128), np.float32), 1)
    iotaE = _rep(np.arange(NE, dtype=np.float32) * CAP + 1)
    tokid = np.ascontiguousarray((np.arange(24)[None, :] * 128 + np.arange(128)[:, None]).astype(np.float32))
    tabinit = np.zeros((128, NSLOT // 128 + 1, 2), np.float32)
    tabinit[:, :, 0] = NOWN
    shared = dict(ident=ident, w_in=f("w_in"), w_pool=f("w_pool"), g2=g2, e2=e2, w_out=f("w_out"),
                  pscale=np.ascontiguousarray(f("pool_scale").reshape(8, 128).T), ln1g=_rep(f("ln1_g")), ln1b=_rep(f("ln1_b")))
    if upto >= 3:
        shared.update(w_xq=f("w_xq"), w_xkv=f("w_xkv"), w_xo=f("w_xo"), ln2g=_rep(f("ln2_g")), ln2b=_rep(f("ln2_b")))
    if upto >= 4:
        shared.update(w_router=f("w_router"), brep=_rep(f("b_router")), triu=triu, iotaE=iotaE, tokid=tokid, tabinit=tabinit)
    if upto >= 5:
        bgu = np.ascontiguousarray(f("b_gu").reshape(NE, 32, 128).transpose(2, 0, 1))
        shared.update(w_gu=f("w_gu")[:n_exp], w_down=f("w_down")[:n_exp], bgu=bgu)
    if upto >= 6:
        shared.update(bdown=f("b_down"), ln3g=_rep(f("ln3_g")), ln3b=_rep(f("ln3_b")))
    maps = []
    for c in range(8):
        pb, ph = c // 2, c % 2
        xs = np.zeros((64, 64, D), np.float32)
        rm = np.zeros((2, 12, 6, 4), np.float32)
        invc = np.zeros((4, 48, 64), np.float32)
        own = []
        for (src, R, cs, nrow, srow0, blk0) in ((x_p[pb].reshape(64, 64, D), 64, 32 * ph, 32, 0, 0),
                                                (x_s[0].reshape(128, 64, D), 128, 16 * c, 16, 40, 8)):
            lo, hi = cs - 4, cs + nrow + 4
            a, b = max(lo, 0), min(hi, R)
            xs[srow0 + (a - lo):srow0 + (b - lo)] = src[a:b]
            own.append(src[cs:cs + nrow].reshape(nrow * 64, D))
            for bb in range(nrow // 4):
                for j in range(12):
                    for t in range(4):
                        r = cs + 4 * bb + t
                        kr = cs + 4 * bb - 4 + j
                        rs = min(max(r - 4, 0), R - 8)
                        ok = (0 <= kr < R) and (rs <= kr <= rs + 7)
                        rm[j % 2, blk0 + bb, j // 2, t] = 0.0 if ok else -1e30
            L = R * 64
            tpos = np.arange(cs * 64, (cs + nrow) * 64)
            orow0 = 0 if blk0 == 0 else 32
            for g, w in enumerate((2, 4, 8, 16)):
                lo_ = np.clip(tpos - w // 2, 0, L)
                hi_ = np.clip(tpos - w // 2 + w, 0, L)
                invc[g, orow0:orow0 + nrow] = (np.float32(1.0) / (hi_ - lo_).astype(np.float32)).reshape(nrow, 64)
        m = dict(shared)
        m.update(xs=xs.reshape(NSLAB, D), rm=np.ascontiguousarray(rm.reshape(2, 288)), invc=np.ascontiguousarray(invc.reshape(4, NOWN)),
                 xown=np.ascontiguousarray(np.concatenate(own, 0)))
        if upto >= 3:
            m["memc"] = np.ascontiguousarray(np.concatenate([mem_p[pb], mem_s[0]], 0))
        maps.append(m)
    return maps


_NC = {}


def kernel(**inputs):
    if "nc" not in _NC:
        _NC["nc"] = build_program()
    nc = _NC["nc"]
    maps = make_inputs(inputs)
    res = run_bass_kernel_spmd(nc, maps, core_ids=list(range(8)))
    y_p = np.empty((4, 4096, D), np.float32)
    y_s = np.empty((1, 8192, D), np.float32)
    for c in range(8):
        o = np.asarray(res.results[c]["out"])
        pb, ph = c // 2, c % 2
        y_p[pb, ph * 2048:(ph + 1) * 2048] = o[:2048]
        y_s[0, c * 1024:(c + 1) * 1024] = o[2048:]
    return (y_p, y_s)
```
